# Optimizing a Trainium2 kernel written in Bass

```python
import math
import jax, jax.numpy as jnp
from jax import lax
import numpy as np

D_MODEL = 1024
BATCH = 8
SEQ = 8192
DEPTH = 2

CHUNK = 64
Q_BLOCK = 128
HEAD_DIM = 64
ROPE_THETA = 10000.0
EPS = 1e-6
A_HEADS = 8
A_LAT = 128
IDX_HEADS = 4
IDX_DIM = 64
TOPK_MAX = 256
B_HEADS = 4
B_VDIM = 2 * HEAD_DIM
C_GROUPS = 8
C_WIDTH = C_GROUPS * HEAD_DIM
SGU_BLOCK = 128
D_WIDTH = 512
CONV_W = 31
D_FF = 2816
PLE_DIM = 256

EVEN_SIZES = (A_HEADS * HEAD_DIM, HEAD_DIM, A_LAT, IDX_HEADS * IDX_DIM, IDX_DIM, IDX_HEADS,
              B_HEADS * 2 * HEAD_DIM, B_HEADS * 2 * HEAD_DIM, B_HEADS * B_VDIM)
EVEN_IN = sum(EVEN_SIZES)
EVEN_OUT = A_HEADS * HEAD_DIM + B_HEADS * B_VDIM
ODD_IN = 2 * C_WIDTH + 2 * D_WIDTH
ODD_OUT = C_WIDTH + D_WIDTH
N_EVEN = (DEPTH + 1) // 2
N_ODD = DEPTH // 2

kernel_name = "chunk_causal_hybrid_dsa_diff_sgu_conv"


def rms_norm(x, g):
    xf = x.astype(jnp.float32)
    y = xf * lax.rsqrt(jnp.mean(xf * xf, axis=-1, keepdims=True) + EPS)
    return (y * g.astype(jnp.float32)).astype(x.dtype)


def layer_norm(x, g, b):
    xf = x.astype(jnp.float32)
    mu = jnp.mean(xf, axis=-1, keepdims=True)
    var = jnp.mean(jnp.square(xf - mu), axis=-1, keepdims=True)
    y = (xf - mu) * lax.rsqrt(var + EPS)
    return (y * g.astype(jnp.float32) + b.astype(jnp.float32)).astype(x.dtype)


def swiglu(x, w_gate, w_up, w_down):
    return (jax.nn.silu(x @ w_gate) * (x @ w_up)) @ w_down


def rope_tables(pos):
    inv = ROPE_THETA ** (-jnp.arange(0, HEAD_DIM, 2, dtype=jnp.float32) / HEAD_DIM)
    ang = pos.astype(jnp.float32)[..., None] * inv
    return jnp.cos(ang), jnp.sin(ang)


def apply_rope(x, cos, sin):
    x1, x2 = jnp.split(x, 2, axis=-1)
    c = cos[:, :, None, :]
    s = sin[:, :, None, :]
    return jnp.concatenate([x1 * c - x2 * s, x2 * c + x1 * s], axis=-1).astype(x.dtype)


def split_cols(z, sizes):
    out, off = [], 0
    for n in sizes:
        out.append(z[..., off:off + n])
        off += n
    return out


def to_blocks(x):
    b, s = x.shape[:2]
    return jnp.moveaxis(x.reshape(b, s // Q_BLOCK, Q_BLOCK, *x.shape[2:]), 1, 0)


def from_blocks(y):
    nb, b, q = y.shape[:3]
    return jnp.moveaxis(y, 0, 1).reshape(b, nb * q, *y.shape[3:])


def dsa_attention(q, k, v_lat, iq, ik, iw, w_uv):
    s_len = q.shape[1]
    top_k = min(TOPK_MAX, s_len // 4)
    key_chunk = jnp.arange(s_len) // CHUNK
    scale = HEAD_DIM ** -0.5
    gather = jax.vmap(lambda a, i: a[i])

    def block(args):
        qb, iqb, iwb, j = args
        q_chunk = (j * Q_BLOCK + jnp.arange(Q_BLOCK)) // CHUNK
        mask = key_chunk[None, :] <= q_chunk[:, None]
        dots = jnp.einsum('bqhe,bse->bqhs', iqb, ik).astype(jnp.float32)
        score = jnp.einsum('bqh,bqhs->bqs', iwb.astype(jnp.float32), jax.nn.relu(dots))
        score = jnp.where(mask, score, -jnp.inf)
        _, sel = lax.top_k(score, top_k)
        valid = (sel // CHUNK) <= q_chunk[None, :, None]
        k_sel = gather(k, sel)
        v_sel = gather(v_lat, sel)
        logits = jnp.einsum('bqhd,bqkd->bhqk', qb, k_sel).astype(jnp.float32) * scale
        logits = jnp.where(valid[:, None], logits, -jnp.inf)
        prob = jax.nn.softmax(logits, axis=-1).astype(v_lat.dtype)
        o_lat = jnp.einsum('bhqk,bqkl->bqhl', prob, v_sel)
        return jnp.einsum('bqhl,hld->bqhd', o_lat, w_uv)

    nb = s_len // Q_BLOCK
    o = lax.map(block, (to_blocks(q), to_blocks(iq), to_blocks(iw), jnp.arange(nb)))
    return from_blocks(o)


def diff_attention(q, k, v, lam, subln_g, lam_init):
    s_len = q.shape[1]
    key_chunk = jnp.arange(s_len) // CHUNK
    scale = HEAD_DIM ** -0.5

    def block(args):
        qb, j = args
        q_chunk = (j * Q_BLOCK + jnp.arange(Q_BLOCK)) // CHUNK
        mask = key_chunk[None, :] <= q_chunk[:, None]
        s = jnp.einsum('bqhcd,bshcd->cbhqs', qb, k).astype(jnp.float32) * scale
        prob = jax.nn.softmax(jnp.where(mask, s, -jnp.inf), axis=-1)
        a = (prob[0] - lam * prob[1]).astype(v.dtype)
        return jnp.einsum('bhqs,bshe->bqhe', a, v)

    nb = s_len // Q_BLOCK
    o = from_blocks(lax.map(block, (to_blocks(q), jnp.arange(nb))))
    return rms_norm(o, subln_g) * (1.0 - lam_init)


def even_mixer(xn, cos, sin, w_in, w_out, a_q_g, a_k_g, a_w_uv, b_q_g, b_k_g,
               lq1, lk1, lq2, lk2, subln_g, lam_init):
    bsz, s_len = xn.shape[:2]
    aq, ak, av, iq, ik, iw, bq, bk, bv = split_cols(xn @ w_in, EVEN_SIZES)
    aq = apply_rope(rms_norm(aq.reshape(bsz, s_len, A_HEADS, HEAD_DIM), a_q_g), cos, sin)
    ak = apply_rope(rms_norm(ak, a_k_g)[:, :, None], cos, sin)[:, :, 0]
    iq = apply_rope(iq.reshape(bsz, s_len, IDX_HEADS, IDX_DIM), cos, sin)
    ik = apply_rope(ik[:, :, None], cos, sin)[:, :, 0]
    a_out = dsa_attention(aq, ak, av, iq, ik, iw, a_w_uv).reshape(bsz, s_len, A_HEADS * HEAD_DIM)
    bq = rms_norm(bq.reshape(bsz, s_len, B_HEADS * 2, HEAD_DIM), b_q_g)
    bk = rms_norm(bk.reshape(bsz, s_len, B_HEADS * 2, HEAD_DIM), b_k_g)
    bq = apply_rope(bq, cos, sin).reshape(bsz, s_len, B_HEADS, 2, HEAD_DIM)
    bk = apply_rope(bk, cos, sin).reshape(bsz, s_len, B_HEADS, 2, HEAD_DIM)
    bv = bv.reshape(bsz, s_len, B_HEADS, B_VDIM)
    lam = (jnp.exp(jnp.sum(lq1.astype(jnp.float32) * lk1.astype(jnp.float32)))
           - jnp.exp(jnp.sum(lq2.astype(jnp.float32) * lk2.astype(jnp.float32))) + lam_init)
    b_out = diff_attention(bq, bk, bv, lam, subln_g, lam_init).reshape(bsz, s_len, B_HEADS * B_VDIM)
    return jnp.concatenate([a_out, b_out], axis=-1) @ w_out


def odd_mixer(xn, w_in, w_out, c_ln_g, c_ln_b, c_w_s, c_b_s, d_conv_w, d_conv_b, d_ln_g, d_ln_b):
    bsz, s_len = xn.shape[:2]
    zc, zd = split_cols(xn @ w_in, (2 * C_WIDTH, 2 * D_WIDTH))
    u, v = jnp.split(jax.nn.gelu(zc), 2, axis=-1)
    v = layer_norm(v, c_ln_g, c_ln_b)
    vb = v.reshape(bsz, s_len // SGU_BLOCK, SGU_BLOCK, C_GROUPS, C_WIDTH // C_GROUPS)
    pc = jnp.arange(SGU_BLOCK) // CHUNK
    w_s = jnp.where((pc[:, None] >= pc[None, :])[None], c_w_s, 0.0)
    sg = jnp.einsum('gij,bnjgc->bnigc', w_s, vb) + c_b_s.T[None, None, :, :, None]
    c_out = u * sg.reshape(bsz, s_len, C_WIDTH)
    a, g = jnp.split(zd, 2, axis=-1)
    hd = a * jax.nn.sigmoid(g)
    hd = lax.conv_general_dilated(hd, d_conv_w[:, None, :], window_strides=(1,),
                                  padding=[(CONV_W - 1, 0)],
                                  dimension_numbers=('NWC', 'WIO', 'NWC'),
                                  feature_group_count=D_WIDTH) + d_conv_b
    d_out = jax.nn.silu(layer_norm(hd, d_ln_g, d_ln_b))
    return jnp.concatenate([c_out, d_out], axis=-1) @ w_out


def setup_inputs(seed: int = 0) -> dict:
    key = jax.random.key(seed)
    ks = list(jax.random.split(key, 48))

    def nrm(shape, scale):
        return jax.random.normal(ks.pop(), shape, jnp.float32) * scale

    def gain(shape):
        return 1.0 + nrm(shape, 0.02)

    D = D_MODEL
    return {
        "x": nrm((BATCH, SEQ, D), 1.0),
        "p": nrm((DEPTH, BATCH, SEQ, PLE_DIM), 1.0),
        "pos": (jax.random.randint(ks.pop(), (BATCH, 1), 0, 4096, dtype=jnp.int32)
                + jnp.arange(SEQ, dtype=jnp.int32)[None, :]).astype(jnp.int32),
        "ffn1_g": gain((DEPTH, D)),
        "ffn1_wg": nrm((DEPTH, D, D_FF), D ** -0.5),
        "ffn1_wu": nrm((DEPTH, D, D_FF), D ** -0.5),
        "ffn1_wd": nrm((DEPTH, D_FF, D), D_FF ** -0.5),
        "mix_g": gain((DEPTH, D)),
        "ffn2_g": gain((DEPTH, D)),
        "ffn2_wg": nrm((DEPTH, D, D_FF), D ** -0.5),
        "ffn2_wu": nrm((DEPTH, D, D_FF), D ** -0.5),
        "ffn2_wd": nrm((DEPTH, D_FF, D), D_FF ** -0.5),
        "ple_g": gain((DEPTH, D)),
        "ple_wgate": nrm((DEPTH, D, D), D ** -0.5),
        "ple_wproj": nrm((DEPTH, PLE_DIM, D), PLE_DIM ** -0.5),
        "ev_w_in": nrm((N_EVEN, D, EVEN_IN), D ** -0.5),
        "ev_w_out": nrm((N_EVEN, EVEN_OUT, D), EVEN_OUT ** -0.5),
        "a_q_g": gain((N_EVEN, HEAD_DIM)),
        "a_k_g": gain((N_EVEN, HEAD_DIM)),
        "a_w_uv": nrm((N_EVEN, A_HEADS, A_LAT, HEAD_DIM), A_LAT ** -0.5),
        "b_q_g": gain((N_EVEN, HEAD_DIM)),
        "b_k_g": gain((N_EVEN, HEAD_DIM)),
        "b_lam_q1": nrm((N_EVEN, HEAD_DIM), 0.1),
        "b_lam_k1": nrm((N_EVEN, HEAD_DIM), 0.1),
        "b_lam_q2": nrm((N_EVEN, HEAD_DIM), 0.1),
        "b_lam_k2": nrm((N_EVEN, HEAD_DIM), 0.1),
        "b_subln_g": gain((N_EVEN, B_VDIM)),
        "od_w_in": nrm((N_ODD, D, ODD_IN), D ** -0.5),
        "od_w_out": nrm((N_ODD, ODD_OUT, D), ODD_OUT ** -0.5),
        "c_ln_g": gain((N_ODD, C_WIDTH)),
        "c_ln_b": nrm((N_ODD, C_WIDTH), 0.02),
        "c_w_s": nrm((N_ODD, C_GROUPS, SGU_BLOCK, SGU_BLOCK), SGU_BLOCK ** -0.5),
        "c_b_s": 1.0 + nrm((N_ODD, C_GROUPS, SGU_BLOCK), 0.1),
        "d_conv_w": nrm((N_ODD, CONV_W, D_WIDTH), CONV_W ** -0.5),
        "d_conv_b": nrm((N_ODD, D_WIDTH), 0.02),
        "d_ln_g": gain((N_ODD, D_WIDTH)),
        "d_ln_b": nrm((N_ODD, D_WIDTH), 0.02),
    }


def reference(x, p, pos, ffn1_g, ffn1_wg, ffn1_wu, ffn1_wd, mix_g, ffn2_g, ffn2_wg, ffn2_wu,
              ffn2_wd, ple_g, ple_wgate, ple_wproj, ev_w_in, ev_w_out, a_q_g, a_k_g, a_w_uv,
              b_q_g, b_k_g, b_lam_q1, b_lam_k1, b_lam_q2, b_lam_k2, b_subln_g, od_w_in,
              od_w_out, c_ln_g, c_ln_b, c_w_s, c_b_s, d_conv_w, d_conv_b, d_ln_g, d_ln_b):
    cos, sin = rope_tables(pos)
    h = x
    for layer in range(DEPTH):
        h = h + 0.5 * swiglu(rms_norm(h, ffn1_g[layer]), ffn1_wg[layer], ffn1_wu[layer], ffn1_wd[layer])
        xn = rms_norm(h, mix_g[layer])
        if layer % 2 == 0:
            e = layer // 2
            lam_init = 0.8 - 0.6 * math.exp(-0.3 * layer)
            mix = even_mixer(xn, cos, sin, ev_w_in[e], ev_w_out[e], a_q_g[e], a_k_g[e], a_w_uv[e],
                             b_q_g[e], b_k_g[e], b_lam_q1[e], b_lam_k1[e], b_lam_q2[e],
                             b_lam_k2[e], b_subln_g[e], lam_init)
        else:
            o = layer // 2
            mix = odd_mixer(xn, od_w_in[o], od_w_out[o], c_ln_g[o], c_ln_b[o], c_w_s[o], c_b_s[o],
                            d_conv_w[o], d_conv_b[o], d_ln_g[o], d_ln_b[o])
        h = h + mix
        h = h + 0.5 * swiglu(rms_norm(h, ffn2_g[layer]), ffn2_wg[layer], ffn2_wu[layer], ffn2_wd[layer])
        gate = jax.nn.sigmoid(rms_norm(h, ple_g[layer]) @ ple_wgate[layer])
        h = h + gate * (p[layer] @ ple_wproj[layer])
    return h
```

```python
from contextlib import ExitStack
import math
import numpy as np
import concourse.bass as bass
import concourse.mybir as mybir
from concourse.bass_utils import run_bass_kernel_spmd

F32 = mybir.dt.float32
BF16 = mybir.dt.bfloat16
I32 = mybir.dt.int32
AF = mybir.ActivationFunctionType
ALU = mybir.AluOpType
AX = mybir.AxisListType

D = 1024
DFF = 2816
NF = DFF // 128
TG = 512
EPS = 1e-6
ENGS = ("pe", "act", "dve", "pool", "sp")
NDMA = 12


class Op:
    __slots__ = ("eng", "fn", "deps", "dma", "sig", "has_dep", "idx")

    def __init__(self, eng, fn, dma):
        self.eng = eng
        self.fn = fn
        self.dma = dma
        self.deps = []
        self.sig = None
        self.has_dep = False


class Sched:
    def __init__(self, nc, es):
        self.nc = nc
        self.cnt = {e: 0 for e in ENGS}
        self.sem = {e: es.enter_context(nc.semaphore("s_" + e)) for e in ENGS}
        self.dsem = {q: [es.enter_context(nc.semaphore("d_%s%d" % (q, i))) for i in range(NDMA)]
                     for q in ("sp", "act", "pool")}
        self.dcnt = {q: 0 for q in ("sp", "act", "pool")}
        self.waited = {}
        self.reset_phase()

    def reset_phase(self):
        self.ops = []
        self.lastw = {}
        self.readers = {}

    def add(self, eng, fn, r=(), w=(), dma=False):
        op = Op(eng, fn, dma)
        op.idx = len(self.ops)
        deps = set()
        for k in r:
            j = self.lastw.get(k)
            if j is not None:
                deps.add(j)
        for k in w:
            j = self.lastw.get(k)
            if j is not None:
                deps.add(j)
            for j in self.readers.get(k, ()):
                deps.add(j)
        for k in r:
            self.readers.setdefault(k, []).append(op.idx)
        for k in w:
            self.lastw[k] = op.idx
            self.readers[k] = []
        deps.discard(op.idx)
        for j in sorted(deps):
            d = self.ops[j]
            if d.eng == "pe" and eng == "pe" and not d.dma and not dma:
                continue
            op.deps.append(j)
            d.has_dep = True
        self.ops.append(op)
        return op

    def dma(self, q, out, in_, r=(), w=(), **kw):
        def fn(e):
            return e.dma_start(out=out, in_=in_, **kw)
        return self.add(q, fn, r, w, dma=True)

    def emit(self):
        nc = self.nc
        ops = self.ops
        last_of = {}
        for op in ops:
            last_of[op.eng] = op
        for op in last_of.values():
            op.has_dep = True
        for op in ops:
            if op.dma:
                q = op.eng
                k = self.dcnt[q]
                self.dcnt[q] += 1
                op.sig = (self.dsem[q][k % NDMA], 16 * (k // NDMA + 1), 16 * (k // NDMA))
            elif op.has_dep:
                self.cnt[op.eng] += 1
                op.sig = (self.sem[op.eng], self.cnt[op.eng], None)
        final = []
        for e in ENGS:
            if self.cnt[e] > 0:
                final.append((self.sem[e], self.cnt[e]))
        for q in self.dsem:
            n = self.dcnt[q]
            for i, s in enumerate(self.dsem[q]):
                if n > i:
                    final.append((s, 16 * ((n - i + NDMA - 1) // NDMA)))
        waited = self.waited

        def wait(e, eobj, sem, val):
            key = (e, id(sem))
            if waited.get(key, 0) >= val:
                return
            waited[key] = val
            eobj.wait_ge(sem, val)

        def run(e):
            def body(eobj):
                for op in ops:
                    if op.eng != e:
                        continue
                    if op.dma and op.sig[2] > 0:
                        wait(e, eobj, op.sig[0], op.sig[2])
                    for j in op.deps:
                        s = ops[j].sig
                        wait(e, eobj, s[0], s[1])
                    ins = op.fn(eobj)
                    if op.sig is not None:
                        ins.then_inc(op.sig[0], 16 if op.dma else 1)
                for (s, v) in final:
                    wait(e, eobj, s, v)
            return body

        with nc.Block() as block:
            block.tensor(run("pe"))
            block.scalar(run("act"))
            block.vector(run("dve"))
            block.gpsimd(run("pool"))
            block.sync(run("sp"))
        self.reset_phase()


C_ID = 0
C_O1024 = 128
C_O512 = 256
C_O128 = 384
C_BD64 = 512
C_ROT = 640
C_ONE = 768
C_SGM = 896
C_INV = 1024
C_SELB = 1028
NCONST = 1028 + 512


def make_consts():
    c = np.zeros((128, NCONST), np.float32)
    c[:, C_ID:C_ID + 128] = np.eye(128, dtype=np.float32)
    c[:, C_O1024:C_O1024 + 128] = 1.0 / 1024
    c[:, C_O512:C_O512 + 128] = 1.0 / 512
    c[:, C_O128:C_O128 + 128] = 1.0 / 128
    bd = np.zeros((128, 128), np.float32)
    bd[:64, :64] = 1.0 / 64
    bd[64:, 64:] = 1.0 / 64
    c[:, C_BD64:C_BD64 + 128] = bd
    P = np.zeros((128, 128), np.float32)
    for hb in (0, 64):
        for i in range(32):
            P[hb + i, hb + i + 32] = -1.0
            P[hb + 32 + i, hb + i] = 1.0
    c[:, C_ROT:C_ROT + 128] = P.T
    c[:, C_ONE:C_ONE + 128] = 1.0
    ii = np.arange(128)
    c[:, C_SGM:C_SGM + 128] = ((ii[:, None] // 64) >= (ii[None, :] // 64)).astype(np.float32)
    inv = (10000.0 ** (-np.arange(0, 64, 2, dtype=np.float32) / 64)).astype(np.float32)
    c[:, C_INV] = inv[ii % 32]
    for gp in range(4):
        for m in range(128):
            c[2 * gp + m // 64, C_SELB + gp * 128 + m] = 1.0
    return c


_UID = [0]


def _uname(name):
    _UID[0] += 1
    return "%s_%d" % (name, _UID[0])


def T(es, nc, name, shape, dt):
    return es.enter_context(nc.sbuf_tensor(_uname(name), shape, dt))


def PS(es, nc, name):
    return es.enter_context(nc.psum_tensor(_uname(name), [128, 512], F32))


def emit_rms(S, cst, hb, hkey, gcol, xn, xkey, sq, sd, rstd, pst, tag):
    ones = cst[:, C_O1024:C_O1024 + 128]
    for c in range(8):
        b = c % 2
        S.add("act", lambda a, c=c, b=b: a.activation(out=sq[b][:], in_=hb[:, c, :], func=AF.Square),
              r=[hkey + (c,)], w=[("sg", b)])
        S.add("pe", lambda t, c=c, b=b: t.matmul(pst[:], lhsT=ones, rhs=sq[b][:], start=(c == 0), stop=(c == 7)),
              r=[("sg", b)], w=[("pst",)])
    S.add("act", lambda a: a.activation(out=sd[:], in_=pst[:], func=AF.Sqrt, bias=EPS, scale=1.0),
          r=[("pst",)], w=[("sd",)])
    S.add("dve", lambda v: v.reciprocal(out=rstd[:], in_=sd[:]), r=[("sd",)], w=[("sd",), ("rstd",)])
    for c in range(8):
        S.add("dve", lambda v, c=c: v.scalar_tensor_tensor(out=xn[:, c, :], in0=hb[:, c, :], scalar=gcol[:, c:c + 1],
                                                            in1=rstd[:], op0=ALU.mult, op1=ALU.mult),
              r=[hkey + (c,), ("rstd",)], w=[xkey + (c,)])


def phase_transpose_in(S, nc, cst, x_d, hT_d, NG):
    with ExitStack() as ph:
        xs = [T(ph, nc, "xs%d" % i, [128, 4, D], F32) for i in range(2)]
        ht = [T(ph, nc, "ht%d" % i, [128, 8, TG], F32) for i in range(2)]
        pt = [PS(ph, nc, "pt%d" % i) for i in range(4)]
        ident = cst[:, C_ID:C_ID + 128]
        xv = x_d.rearrange("(g s p) d -> g p s d", s=4, p=128)
        for g in range(NG):
            sl = g % 2
            S.dma("sp", xs[sl][:], xv[g], w=[("xs", sl)])
            for c in range(8):
                b = c % 4
                def tr(t, c=c, b=b, sl=sl):
                    for s in range(4):
                        ins = t.transpose(out=pt[b][:, s * 128:(s + 1) * 128], in_=xs[sl][:, s, c * 128:(c + 1) * 128],
                                          identity=ident)
                    return ins
                S.add("pe", tr, r=[("xs", sl)], w=[("pt", b)])
                if c % 2 == 0:
                    S.add("act", lambda a, c=c, b=b, sl=sl: a.copy(out=ht[sl][:, c, :], in_=pt[b][:]),
                          r=[("pt", b)], w=[("ht", sl, c)])
                else:
                    S.add("dve", lambda v, c=c, b=b, sl=sl: v.tensor_copy(out=ht[sl][:, c, :], in_=pt[b][:]),
                          r=[("pt", b)], w=[("ht", sl, c)])
            S.dma("pool", hT_d[:, :, g * TG:(g + 1) * TG], ht[sl][:], r=[("ht", sl, c) for c in range(8)], w=[("hTd", g)])
        S.emit()


def phase_ffn(S, nc, cst, gcol, hT_d, wg_d, wu_d, wd_d, NG):
    with ExitStack() as ph:
        Wg = T(ph, nc, "Wg", [128, 8, DFF], BF16)
        Wu = T(ph, nc, "Wu", [128, 8, DFF], BF16)
        Wd = T(ph, nc, "Wd", [128, NF, D], BF16)
        hb = [T(ph, nc, "hb%d" % i, [128, 8, TG], F32) for i in range(2)]
        xn = T(ph, nc, "xn", [128, 8, TG], BF16)
        act = T(ph, nc, "act", [128, NF, TG], BF16)
        sd = T(ph, nc, "sd", [128, TG], F32)
        rstd = sd
        sg = [T(ph, nc, "sg%d" % i, [128, TG], F32) for i in range(2)]
        sq = sg
        pst = PS(ph, nc, "pst")
        pg = [PS(ph, nc, "pg%d" % i) for i in range(2)]
        pu = [PS(ph, nc, "pu%d" % i) for i in range(2)]
        po = [PS(ph, nc, "po%d" % i) for i in range(2)]
        wgv = wg_d.rearrange("(c p) f -> p c f", p=128)
        wuv_ = wu_d.rearrange("(c p) f -> p c f", p=128)
        FB = [0, 256, 1024, 1920, DFF]
        for k in range(4):
            S.dma("pool", Wg[:, :, FB[k]:FB[k + 1]], wgv[:, :, FB[k]:FB[k + 1]], w=[("Wg", k)])
            S.dma("pool", Wu[:, :, FB[k]:FB[k + 1]], wuv_[:, :, FB[k]:FB[k + 1]], w=[("Wu", k)])
        for f in range(NF):
            S.dma("pool", Wd[:, f, :], wd_d[f * 128:(f + 1) * 128, :], w=[("Wd", f)])

        def fblk(f):
            for k in range(4):
                if f * 128 < FB[k + 1]:
                    return k
        S.dma("sp", hb[0][:], hT_d[:, :, 0:TG], w=[("h", 0, c) for c in range(8)])
        for g in range(NG):
            sl = g % 2
            if g + 1 < NG:
                S.dma("sp", hb[1 - sl][:], hT_d[:, :, (g + 1) * TG:(g + 2) * TG], w=[("h", 1 - sl, c) for c in range(8)])
            emit_rms(S, cst, hb[sl], ("h", sl), gcol, xn, ("xn",), sq, sd, rstd, pst, "f")
            xr = [("xn", c) for c in range(8)]
            for f in range(NF):
                b = f % 2
                def mmg(t, f=f, b=b):
                    for c in range(8):
                        ins = t.matmul(pg[b][:], lhsT=Wg[:, c, f * 128:(f + 1) * 128], rhs=xn[:, c, :],
                                       start=(c == 0), stop=(c == 7))
                    return ins
                def mmu(t, f=f, b=b):
                    for c in range(8):
                        ins = t.matmul(pu[b][:], lhsT=Wu[:, c, f * 128:(f + 1) * 128], rhs=xn[:, c, :],
                                       start=(c == 0), stop=(c == 7))
                    return ins
                S.add("pe", mmg, r=xr + [("Wg", fblk(f))], w=[("pg", b)])
                S.add("pe", mmu, r=xr + [("Wu", fblk(f))], w=[("pu", b)])
                S.add("act", lambda a, b=b: a.activation(out=sg[b][:], in_=pg[b][:], func=AF.Silu),
                      r=[("pg", b)], w=[("sg", b)])
                S.add("dve", lambda v, f=f, b=b: v.tensor_tensor(out=act[:, f, :], in0=pu[b][:], in1=sg[b][:], op=ALU.mult),
                      r=[("pu", b), ("sg", b)], w=[("act", f)])
            ar = [("act", f) for f in range(NF)]
            for m in range(8):
                b = m % 2
                def mmd(t, m=m, b=b):
                    for f in range(NF):
                        ins = t.matmul(po[b][:], lhsT=Wd[:, f, m * 128:(m + 1) * 128], rhs=act[:, f, :],
                                       start=(f == 0), stop=(f == NF - 1))
                    return ins
                S.add("pe", mmd, r=ar + [("Wd", f) for f in range(NF)], w=[("po", b)])
                S.add("dve", lambda v, m=m, b=b, sl=sl: v.scalar_tensor_tensor(
                    out=hb[sl][:, m, :], in0=po[b][:], scalar=0.5, in1=hb[sl][:, m, :], op0=ALU.mult, op1=ALU.add),
                    r=[("po", b), ("h", sl, m)], w=[("h", sl, m)])
            S.dma("pool", hT_d[:, :, g * TG:(g + 1) * TG], hb[sl][:], r=[("h", sl, c) for c in range(8)], w=[("hTd", g)])
        S.emit()


def emit_rope_tables(S, nc, cst, posb_d, g, posi, ang, tmpf, tmpi, Ct, St):
    TWO_PI = 2.0 * math.pi
    S.dma("sp", posi[:], posb_d[:, g * TG:(g + 1) * TG], w=[("posi",)])
    S.add("dve", lambda v: v.tensor_copy(out=ang[:], in_=posi[:]), r=[("posi",)], w=[("ang",)])
    S.add("dve", lambda v: v.tensor_scalar(out=ang[:], in0=ang[:], scalar1=cst[:, C_INV:C_INV + 1], scalar2=None,
                                           op0=ALU.mult), r=[("ang",)], w=[("ang",)])
    for (dst, key, shift) in ((St, "St", 0.0), (Ct, "Ct", 0.5 * math.pi)):
        S.add("dve", lambda v, shift=shift: v.tensor_scalar(out=tmpi[:], in0=ang[:], scalar1=shift, scalar2=1.0 / TWO_PI,
                                                            op0=ALU.add, op1=ALU.mult), r=[("ang",)], w=[("tmpi",)])
        S.add("dve", lambda v: v.tensor_copy(out=tmpf[:], in_=tmpi[:]), r=[("tmpi",)], w=[("tmpf",)])
        S.add("dve", lambda v: v.scalar_tensor_tensor(out=tmpf[:], in0=tmpf[:], scalar=-TWO_PI, in1=ang[:],
                                                      op0=ALU.mult, op1=ALU.add), r=[("tmpf",), ("ang",)], w=[("tmpf",)])
        S.add("dve", lambda v, shift=shift: v.tensor_scalar(out=tmpf[:], in0=tmpf[:], scalar1=shift, scalar2=None,
                                                            op0=ALU.add), r=[("tmpf",)], w=[("tmpf",)])
        S.add("dve", lambda v, dst=dst: v.tensor_scalar(out=dst[:], in0=tmpf[:], scalar1=math.pi, scalar2=-TWO_PI,
                                                        op0=ALU.is_gt, op1=ALU.mult), r=[("tmpf",)], w=[(key,)])
        S.add("dve", lambda v, dst=dst: v.tensor_tensor(out=dst[:], in0=dst[:], in1=tmpf[:], op=ALU.add),
              r=[("tmpf",), (key,)], w=[(key,)])
        S.add("dve", lambda v, dst=dst: v.tensor_scalar(out=dst[:], in0=dst[:], scalar1=-math.pi, scalar2=math.pi,
                                                        op0=ALU.max, op1=ALU.min), r=[(key,)], w=[(key,)])
        S.add("act", lambda a, dst=dst: a.activation(out=dst[:], in_=dst[:], func=AF.Sin), r=[(key,)], w=[(key,)])


def phase_evenproj(S, nc, cst, gcol, hgc, hT_d, posb_d, wfm_d, wtm_d, qkT_d, bv_d, av_d, iw_d, NG):
    with ExitStack() as ph:
        Wfm = T(ph, nc, "Wfm", [128, 8, 2048], BF16)
        Wtm = T(ph, nc, "Wtm", [128, 8, 644], BF16)
        hb = [T(ph, nc, "hb%d" % i, [128, 8, TG], F32) for i in range(2)]
        xn = T(ph, nc, "xn", [128, 8, TG], BF16)
        sd = T(ph, nc, "sd", [128, TG], F32)
        sg = [T(ph, nc, "sg%d" % i, [128, TG], F32) for i in range(2)]
        posi = T(ph, nc, "posi", [128, TG], I32)
        ang = T(ph, nc, "ang", [128, TG], F32)
        tmpf = T(ph, nc, "tmpf", [128, TG], F32)
        tmpi = T(ph, nc, "tmpi", [128, TG], I32)
        Ct = T(ph, nc, "Ct", [128, TG], F32)
        St = T(ph, nc, "St", [128, TG], F32)
        sqb = [T(ph, nc, "sqb%d" % i, [128, TG], F32) for i in range(3)]
        sdb = [T(ph, nc, "sdb%d" % i, [128, TG], F32) for i in range(3)]
        xb = [T(ph, nc, "xb%d" % i, [128, TG], F32) for i in range(3)]
        t1 = [T(ph, nc, "t1%d" % i, [128, TG], F32) for i in range(2)]
        t2 = [T(ph, nc, "t2%d" % i, [128, TG], F32) for i in range(2)]
        ob = [T(ph, nc, "ob%d" % i, [128, TG], BF16) for i in range(3)]
        bvs = [T(ph, nc, "bvs%d" % i, [128, 4, 512], BF16) for i in range(2)]
        avs = [T(ph, nc, "avs%d" % i, [128, 4, 128], BF16) for i in range(2)]
        iws = [T(ph, nc, "iws%d" % i, [128, 4, 4], F32) for i in range(2)]
        pst = PS(ph, nc, "pst")
        pta = PS(ph, nc, "pta")
        ptb = PS(ph, nc, "ptb")
        pz = [PS(ph, nc, "pz%d" % i) for i in range(3)]
        pms = PS(ph, nc, "pms")
        prot = PS(ph, nc, "prot")
        bd64 = cst[:, C_BD64:C_BD64 + 128]
        rotT = cst[:, C_ROT:C_ROT + 128]
        for c in range(8):
            S.dma("pool", Wfm[:, c, :], wfm_d[c * 128:(c + 1) * 128, :], w=[("Wfm", c)])
            S.dma("pool", Wtm[:, c, :], wtm_d[c * 128:(c + 1) * 128, :], w=[("Wtm", c)])
        wfr = [("Wfm", c) for c in range(8)]
        wtr = [("Wtm", c) for c in range(8)]
        bvv = bv_d.rearrange("(g s p) e -> g p s e", s=4, p=128)
        avv = av_d.rearrange("(g s p) e -> g p s e", s=4, p=128)
        iwv = iw_d.rearrange("(g s p) e -> g p s e", s=4, p=128)
        gidx = [0, 0, 0, 0, 1, None, None, None, 2, 2, 2, 2, 3, 3, 3, 3]
        S.dma("sp", hb[0][:], hT_d[:, :, 0:TG], w=[("h", 0, c) for c in range(8)])
        for g in range(NG):
            sl = g % 2
            if g + 1 < NG:
                S.dma("sp", hb[1 - sl][:], hT_d[:, :, (g + 1) * TG:(g + 2) * TG], w=[("h", 1 - sl, c) for c in range(8)])
            emit_rope_tables(S, nc, cst, posb_d, g, posi, ang, tmpf, tmpi, Ct, St)
            emit_rms(S, cst, hb[sl], ("h", sl), gcol, xn, ("xn",), sg, sd, sd, pst, "b")
            xr = [("xn", c) for c in range(8)]
            for s4 in range(4):
                def mma(t, s4=s4):
                    for c in range(8):
                        ins = t.matmul(pta[:], lhsT=xn[:, c, s4 * 128:(s4 + 1) * 128], rhs=Wtm[:, c, 0:512],
                                       start=(c == 0), stop=(c == 7))
                    return ins
                def mmb(t, s4=s4):
                    for c in range(8):
                        ins = t.matmul(ptb[:, 0:132], lhsT=xn[:, c, s4 * 128:(s4 + 1) * 128], rhs=Wtm[:, c, 512:644],
                                       start=(c == 0), stop=(c == 7))
                    return ins
                S.add("pe", mma, r=xr + wtr, w=[("pta",)])
                S.add("pe", mmb, r=xr + wtr, w=[("ptb",)])
                S.add("act", lambda a, s4=s4, sl=sl: a.copy(out=bvs[sl][:, s4, :], in_=pta[:]), r=[("pta",)], w=[("bvs", sl, s4)])
                S.add("dve", lambda v, s4=s4, sl=sl: v.tensor_copy(out=avs[sl][:, s4, :], in_=ptb[:, 0:128]),
                      r=[("ptb",)], w=[("avs", sl, s4)])
                S.add("dve", lambda v, s4=s4, sl=sl: v.tensor_copy(out=iws[sl][:, s4, :], in_=ptb[:, 128:132]),
                      r=[("ptb",)], w=[("iws", sl, s4)])
            S.dma("pool", bvv[g], bvs[sl][:], r=[("bvs", sl, k) for k in range(4)], w=[("bvd", g)])
            S.dma("pool", avv[g], avs[sl][:], r=[("avs", sl, k) for k in range(4)], w=[("avd", g)])
            S.dma("pool", iwv[g], iws[sl][:], r=[("iws", sl, k) for k in range(4)], w=[("iwd", g)])
            def stageA(ci):
                b = ci % 3
                def mmz(t, ci=ci, b=b):
                    for c in range(8):
                        ins = t.matmul(pz[b][:], lhsT=Wfm[:, c, ci * 128:(ci + 1) * 128], rhs=xn[:, c, :],
                                       start=(c == 0), stop=(c == 7))
                    return ins
                S.add("pe", mmz, r=xr + wfr, w=[("pz", b)])
                if gidx[ci] is not None:
                    S.add("act", lambda a, b=b: a.activation(out=sqb[b][:], in_=pz[b][:], func=AF.Square),
                          r=[("pz", b)], w=[("sqb", b)])
                else:
                    S.add("act", lambda a, b=b: a.copy(out=xb[b][:], in_=pz[b][:]), r=[("pz", b)], w=[("xb", b)])

            def stageB(ci):
                b = ci % 3
                if gidx[ci] is None:
                    return
                gi = gidx[ci]
                S.add("pe", lambda t, b=b: t.matmul(pms[:], lhsT=bd64, rhs=sqb[b][:], start=True, stop=True),
                      r=[("sqb", b)], w=[("pms",)])
                S.add("act", lambda a, b=b: a.activation(out=sdb[b][:], in_=pms[:], func=AF.Sqrt, bias=EPS, scale=1.0),
                      r=[("pms",)], w=[("sdb", b)])
                S.add("dve", lambda v, b=b: v.reciprocal(out=sdb[b][:], in_=sdb[b][:]), r=[("sdb", b)], w=[("sdb", b)])
                S.add("dve", lambda v, b=b, gi=gi: v.scalar_tensor_tensor(
                    out=xb[b][:], in0=pz[b][:], scalar=hgc[:, gi:gi + 1], in1=sdb[b][:], op0=ALU.mult, op1=ALU.mult),
                    r=[("pz", b), ("sdb", b)], w=[("xb", b)])

            def stageC(ci, g=g):
                b = ci % 3
                b2 = ci % 2
                S.add("pe", lambda t, b=b: t.matmul(prot[:], lhsT=rotT, rhs=xb[b][:], start=True, stop=True),
                      r=[("xb", b)], w=[("prot",)])
                S.add("pool", lambda v, b=b, b2=b2: v.tensor_tensor(out=t1[b2][:], in0=xb[b][:], in1=Ct[:], op=ALU.mult),
                      r=[("xb", b), ("Ct",)], w=[("t1", b2)])
                S.add("dve", lambda v, b2=b2: v.tensor_tensor(out=t2[b2][:], in0=prot[:], in1=St[:], op=ALU.mult),
                      r=[("prot",), ("St",)], w=[("t2", b2)])
                S.add("pool", lambda v, b=b, b2=b2: v.tensor_tensor(out=ob[b][:], in0=t1[b2][:], in1=t2[b2][:], op=ALU.add),
                      r=[("t1", b2), ("t2", b2)], w=[("ob", b)])
                S.dma("pool", qkT_d[ci, :, g * TG:(g + 1) * TG], ob[b][:], r=[("ob", b)], w=[("qkd", ci, g)])

            for step in range(18):
                if step < 16:
                    stageA(step)
                if 0 <= step - 1 < 16:
                    stageB(step - 1)
                if 0 <= step - 2 < 16:
                    stageC(step - 2)
        S.emit()


def phase_proj_res(S, nc, hT_d, catT_d, w_d, NG):
    with ExitStack() as ph:
        W = T(ph, nc, "Wo", [128, 8, D], BF16)
        hb = [T(ph, nc, "hb%d" % i, [128, 8, TG], F32) for i in range(2)]
        cat = [T(ph, nc, "cat%d" % i, [128, 8, TG], BF16) for i in range(2)]
        po = [PS(ph, nc, "po%d" % i) for i in range(2)]
        for c in range(8):
            S.dma("pool", W[:, c, :], w_d[c * 128:(c + 1) * 128, :], w=[("W", c)])
        cv = catT_d.rearrange("c p t -> p c t")
        for g in range(NG):
            sl = g % 2
            S.dma("sp", hb[sl][:], hT_d[:, :, g * TG:(g + 1) * TG], w=[("h", sl, c) for c in range(8)])
            S.dma("sp", cat[sl][:], cv[:, :, g * TG:(g + 1) * TG], w=[("cat", sl)])
            for m in range(8):
                b = m % 2
                def mm(t, m=m, b=b, sl=sl):
                    for c in range(8):
                        ins = t.matmul(po[b][:], lhsT=W[:, c, m * 128:(m + 1) * 128], rhs=cat[sl][:, c, :],
                                       start=(c == 0), stop=(c == 7))
                    return ins
                S.add("pe", mm, r=[("cat", sl)] + [("W", c) for c in range(8)], w=[("po", b)])
                S.add("dve", lambda v, m=m, b=b, sl=sl: v.tensor_tensor(out=hb[sl][:, m, :], in0=po[b][:], in1=hb[sl][:, m, :],
                                                                        op=ALU.add),
                      r=[("po", b), ("h", sl, m)], w=[("h", sl, m)])
            S.dma("pool", hT_d[:, :, g * TG:(g + 1) * TG], hb[sl][:], r=[("h", sl, c) for c in range(8)], w=[("hTd", g)])
        S.emit()


def phase_ple(S, nc, cst, gcol, hT_d, p_d, wgate_d, wproj_d, out_d, NG):
    with ExitStack() as ph:
        Wg = T(ph, nc, "Wgt", [128, 8, D], BF16)
        Wp = T(ph, nc, "Wpj", [128, 2, D], BF16)
        hb = [T(ph, nc, "hb%d" % i, [128, 8, TG], F32) for i in range(2)]
        xn = T(ph, nc, "xn", [128, 8, TG], BF16)
        sd = T(ph, nc, "sd", [128, TG], F32)
        sg = [T(ph, nc, "sg%d" % i, [128, TG], F32) for i in range(2)]
        ps_ = [T(ph, nc, "ps%d" % i, [128, 4, 256], F32) for i in range(2)]
        pT = T(ph, nc, "pT", [128, 2, TG], BF16)
        tt = [T(ph, nc, "tt%d" % i, [128, TG], F32) for i in range(2)]
        if out_d is not None:
            os_ = [T(ph, nc, "os%d" % i, [128, 4, D], F32) for i in range(2)]
        pst = PS(ph, nc, "pst")
        pg = [PS(ph, nc, "pg%d" % i) for i in range(2)]
        pp = [PS(ph, nc, "pp%d" % i) for i in range(2)]
        ptr = [PS(ph, nc, "ptr%d" % i) for i in range(2)]
        ident = cst[:, C_ID:C_ID + 128]
        for c in range(8):
            S.dma("pool", Wg[:, c, :], wgate_d[c * 128:(c + 1) * 128, :], w=[("Wg", c)])
        for c in range(2):
            S.dma("pool", Wp[:, c, :], wproj_d[c * 128:(c + 1) * 128, :], w=[("Wp", c)])
        pv = p_d.rearrange("(g s p) e -> g p s e", s=4, p=128)
        if out_d is not None:
            ov = out_d.rearrange("(g s p) d -> g p s d", s=4, p=128)
        for g in range(NG):
            sl = g % 2
            S.dma("sp", hb[sl][:], hT_d[:, :, g * TG:(g + 1) * TG], w=[("h", sl, c) for c in range(8)])
            S.dma("sp", ps_[sl][:], pv[g], w=[("ps", sl)])
            for c2 in range(2):
                def tr(t, c2=c2, sl=sl):
                    for s4 in range(4):
                        ins = t.transpose(out=ptr[c2][:, s4 * 128:(s4 + 1) * 128], in_=ps_[sl][:, s4, c2 * 128:(c2 + 1) * 128],
                                          identity=ident)
                    return ins
                S.add("pe", tr, r=[("ps", sl)], w=[("ptr", c2)])
                S.add("act", lambda a, c2=c2: a.copy(out=pT[:, c2, :], in_=ptr[c2][:]), r=[("ptr", c2)], w=[("pT", c2)])
            emit_rms(S, cst, hb[sl], ("h", sl), gcol, xn, ("xn",), sg, sd, sd, pst, "p")
            xr = [("xn", c) for c in range(8)]
            for m in range(8):
                b = m % 2
                def mg(t, m=m, b=b):
                    for c in range(8):
                        ins = t.matmul(pg[b][:], lhsT=Wg[:, c, m * 128:(m + 1) * 128], rhs=xn[:, c, :],
                                       start=(c == 0), stop=(c == 7))
                    return ins
                def mp(t, m=m, b=b):
                    for c in range(2):
                        ins = t.matmul(pp[b][:], lhsT=Wp[:, c, m * 128:(m + 1) * 128], rhs=pT[:, c, :],
                                       start=(c == 0), stop=(c == 1))
                    return ins
                S.add("pe", mg, r=xr + [("Wg", c) for c in range(8)], w=[("pg", b)])
                S.add("pe", mp, r=[("pT", 0), ("pT", 1), ("Wp", 0), ("Wp", 1)], w=[("pp", b)])
                S.add("act", lambda a, b=b: a.activation(out=sg[b][:], in_=pg[b][:], func=AF.Sigmoid),
                      r=[("pg", b)], w=[("sg", b)])
                S.add("dve", lambda v, b=b: v.tensor_tensor(out=tt[b][:], in0=pp[b][:], in1=sg[b][:], op=ALU.mult),
                      r=[("pp", b), ("sg", b)], w=[("tt", b)])
                S.add("dve", lambda v, m=m, b=b, sl=sl: v.tensor_tensor(out=hb[sl][:, m, :], in0=tt[b][:], in1=hb[sl][:, m, :],
                                                                        op=ALU.add),
                      r=[("tt", b), ("h", sl, m)], w=[("h", sl, m)])
            if out_d is None:
                S.dma("pool", hT_d[:, :, g * TG:(g + 1) * TG], hb[sl][:], r=[("h", sl, c) for c in range(8)], w=[("hTd", g)])
            else:
                for s4 in range(4):
                    for half in range(2):
                        b = half
                        def trb(t, s4=s4, half=half, b=b, sl=sl):
                            for k in range(4):
                                c = half * 4 + k
                                ins = t.transpose(out=ptr[b][:, k * 128:(k + 1) * 128],
                                                  in_=hb[sl][:, c, s4 * 128:(s4 + 1) * 128], identity=ident)
                            return ins
                        S.add("pe", trb, r=[("h", sl, half * 4 + k) for k in range(4)], w=[("ptr", b)])
                        if half == 0:
                            S.add("act", lambda a, s4=s4, b=b, sl=sl: a.copy(out=os_[sl][:, s4, 0:512], in_=ptr[b][:]),
                                  r=[("ptr", b)], w=[("os", sl, s4, 0)])
                        else:
                            S.add("dve", lambda v, s4=s4, b=b, sl=sl: v.tensor_copy(out=os_[sl][:, s4, 512:1024], in_=ptr[b][:]),
                                  r=[("ptr", b)], w=[("os", sl, s4, 1)])
                S.dma("pool", ov[g], os_[sl][:], r=[("os", sl, a, b_) for a in range(4) for b_ in range(2)], w=[("outd", g)])
        S.emit()


def phase_diff(S, nc, cst, hgc, lamv_d, qkT_d, bv_d, catT_d, S_LEN):
    NT = S_LEN // 128
    NG = S_LEN // TG
    lam_init = 0.8 - 0.6 * math.exp(-0.3 * 0)
    U32 = mybir.dt.uint32
    with ExitStack() as ph:
        kT = T(ph, nc, "kT", [128, S_LEN], BF16)
        vT = T(ph, nc, "vT", [128, NT, 128], BF16)
        qT = [T(ph, nc, "qT%d" % i, [128, TG], BF16) for i in range(2)]
        E = [[T(ph, nc, "E%d%d" % (cm, b), [128, TG], BF16) for b in range(2)] for cm in range(2)]
        onesb = T(ph, nc, "onesb", [128, 128], BF16)
        lamv = T(ph, nc, "lamv", [128, 4, 64], F32)
        lt = T(ph, nc, "lt", [128, 2, 64], F32)
        ls = T(ph, nc, "ls", [128, 2], F32)
        neglam = T(ph, nc, "neglam", [128, 1], F32)
        gsub = T(ph, nc, "gsub", [128, 1], F32)
        rd = [T(ph, nc, "rd%d" % i, [128, TG], F32) for i in range(2)]
        t0 = T(ph, nc, "t0", [128, TG], F32)
        t1 = T(ph, nc, "t1", [128, TG], F32)
        oo = T(ph, nc, "oo", [128, TG], F32)
        sq = T(ph, nc, "sq", [128, TG], F32)
        sdd = T(ph, nc, "sdd", [128, TG], F32)
        obf = [T(ph, nc, "obf%d" % i, [128, TG], BF16) for i in range(2)]
        pl = [[PS(ph, nc, "pl%d%d" % (cm, b)) for b in range(2)] for cm in range(2)]
        pO = [PS(ph, nc, "pO%d" % i) for i in range(2)]
        pD = [PS(ph, nc, "pD%d" % i) for i in range(2)]
        o128 = cst[:, C_O128:C_O128 + 128]
        S.dma("sp", lamv[:], lamv_d, w=[("lamv",)])
        S.add("dve", lambda v: v.memset(onesb[:], 1.0), w=[("onesb",)])
        S.add("dve", lambda v: v.tensor_tensor(out=lt[:, 0, :], in0=lamv[:, 0, :], in1=lamv[:, 1, :], op=ALU.mult),
              r=[("lamv",)], w=[("lt", 0)])
        S.add("dve", lambda v: v.tensor_tensor(out=lt[:, 1, :], in0=lamv[:, 2, :], in1=lamv[:, 3, :], op=ALU.mult),
              r=[("lamv",)], w=[("lt", 1)])
        S.add("dve", lambda v: v.reduce_sum(out=ls[:, 0:1], in_=lt[:, 0, :], axis=AX.X), r=[("lt", 0)], w=[("ls", 0)])
        S.add("dve", lambda v: v.reduce_sum(out=ls[:, 1:2], in_=lt[:, 1, :], axis=AX.X), r=[("lt", 1)], w=[("ls", 1)])
        S.add("act", lambda a: a.activation(out=ls[:], in_=ls[:], func=AF.Exp), r=[("ls", 0), ("ls", 1)], w=[("ls", 0), ("ls", 1)])
        S.add("dve", lambda v: v.tensor_tensor(out=neglam[:], in0=ls[:, 1:2], in1=ls[:, 0:1], op=ALU.subtract),
              r=[("ls", 0), ("ls", 1)], w=[("neglam",)])
        S.add("dve", lambda v: v.tensor_scalar(out=neglam[:], in0=neglam[:], scalar1=-lam_init, scalar2=None, op0=ALU.add),
              r=[("neglam",)], w=[("neglam",)])
        S.add("dve", lambda v: v.tensor_scalar(out=gsub[:], in0=hgc[:, 4:5], scalar1=1.0 - lam_init, scalar2=None, op0=ALU.mult),
              r=[("hgc",)], w=[("gsub",)])
        bvv = bv_d.rearrange("(n p) e -> p n e", p=128)
        for h in range(4):
            S.dma("sp", kT[:], qkT_d[12 + h], w=[("kT",)])
            for n0 in range(0, NT, 8):
                n1 = min(NT, n0 + 8)
                S.dma("sp", vT[:, n0:n1, :], bvv[:, n0:n1, h * 128:(h + 1) * 128], w=[("vT", n0)])
            for qg in range(NG):
                qs = qg % 2
                S.dma("sp", qT[qs][:], qkT_d[8 + h, :, qg * TG:(qg + 1) * TG], w=[("qT", qs)])
                nkt = 4 * qg + 4

                def emitL(kt, qg=qg, qs=qs):
                    r_ = kt - 4 * qg
                    c0 = max(0, r_) * 128
                    b = kt % 2
                    for cm in range(2):
                        S.add("pe", lambda t, cm=cm, kt=kt, c0=c0, b=b: t.matmul(
                            pl[cm][b][:, c0:TG], lhsT=kT[cm * 64:(cm + 1) * 64, kt * 128:(kt + 1) * 128],
                            rhs=qT[qs][cm * 64:(cm + 1) * 64, c0:TG], start=True, stop=True),
                            r=[("kT",), ("qT", qs)], w=[("pl", cm, b)])
                        S.add("act", lambda a, cm=cm, c0=c0, b=b: a.activation(
                            out=E[cm][b][:, c0:TG], in_=pl[cm][b][:, c0:TG], func=AF.Exp, scale=0.125),
                            r=[("pl", cm, b)], w=[("E", cm, b)])
                        if r_ >= 0:
                            S.add("pool", lambda p_, cm=cm, c0=c0, b=b: p_.memset(E[cm][b][64:128, c0:c0 + 64], 0.0),
                                  r=[("E", cm, b)], w=[("E", cm, b)])

                def emitPV(kt, qg=qg, nkt=nkt):
                    c0 = max(0, kt - 4 * qg) * 128
                    b = kt % 2
                    for cm in range(2):
                        def pv(t, cm=cm, kt=kt, c0=c0, b=b):
                            t.matmul(pO[cm][:, c0:TG], lhsT=vT[:, kt, :], rhs=E[cm][b][:, c0:TG],
                                     start=(kt == 0), stop=(kt == nkt - 1))
                            return t.matmul(pD[cm][:, c0:TG], lhsT=onesb[:], rhs=E[cm][b][:, c0:TG],
                                            start=(kt == 0), stop=(kt == nkt - 1))
                        S.add("pe", pv, r=[("E", cm, b), ("vT", (kt // 8) * 8), ("onesb",)], w=[("pO", cm), ("pD", cm)])

                emitL(0)
                for kt in range(nkt):
                    if kt + 1 < nkt:
                        emitL(kt + 1)
                    emitPV(kt)
                for cm in range(2):
                    S.add("dve", lambda v, cm=cm: v.reciprocal(out=rd[cm][:], in_=pD[cm][:]), r=[("pD", cm)], w=[("rd", cm)])
                S.add("dve", lambda v: v.tensor_tensor(out=t0[:], in0=pO[0][:], in1=rd[0][:], op=ALU.mult),
                      r=[("pO", 0), ("rd", 0)], w=[("t0",)])
                S.add("dve", lambda v: v.tensor_scalar(out=rd[1][:], in0=rd[1][:], scalar1=neglam[:, 0:1], scalar2=None, op0=ALU.mult),
                      r=[("rd", 1), ("neglam",)], w=[("rd", 1)])
                S.add("dve", lambda v: v.tensor_tensor(out=t1[:], in0=pO[1][:], in1=rd[1][:], op=ALU.mult),
                      r=[("pO", 1), ("rd", 1)], w=[("t1",)])
                S.add("dve", lambda v: v.tensor_tensor(out=oo[:], in0=t0[:], in1=t1[:], op=ALU.add),
                      r=[("t0",), ("t1",)], w=[("oo",)])
                S.add("act", lambda a: a.activation(out=sq[:], in_=oo[:], func=AF.Square), r=[("oo",)], w=[("sq",)])
                S.add("pe", lambda t: t.matmul(pl[0][0][:], lhsT=o128, rhs=sq[:], start=True, stop=True),
                      r=[("sq",)], w=[("pl", 0, 0)])
                S.add("act", lambda a: a.activation(out=sdd[:], in_=pl[0][0][:], func=AF.Sqrt, bias=EPS, scale=1.0),
                      r=[("pl", 0, 0)], w=[("sdd",)])
                S.add("dve", lambda v: v.reciprocal(out=sdd[:], in_=sdd[:]), r=[("sdd",)], w=[("sdd",)])
                S.add("dve", lambda v, qs=qs: v.scalar_tensor_tensor(out=obf[qs][:], in0=oo[:], scalar=gsub[:, 0:1], in1=sdd[:],
                                                                     op0=ALU.mult, op1=ALU.mult),
                      r=[("oo",), ("sdd",), ("gsub",)], w=[("obf", qs)])
                S.dma("pool", catT_d[4 + h, :, qg * TG:(qg + 1) * TG], obf[qs][:], r=[("obf", qs)], w=[("catd", h, qg)])
        S.emit()


def phase_dsa(S, nc, cst, qkT_d, av_d, iw_d, wuv_d, catT_d, S_LEN):
    NT = S_LEN // 128
    NITER = 14
    TOPK = 256.0
    BIGM = 30000.0
    with ExitStack() as ph:
        akT = T(ph, nc, "akT", [128, S_LEN], BF16)
        ikT = T(ph, nc, "ikT", [128, S_LEN], BF16)
        vT = T(ph, nc, "avT", [128, NT, 128], BF16)
        wuv = T(ph, nc, "wuv", [128, 8, 128], BF16)
        irep = T(ph, nc, "irep", [128, 512], BF16)
        onesb = T(ph, nc, "onesb2", [128, 128], BF16)
        sc = T(ph, nc, "sc", [128, S_LEN], F32)
        mb = [T(ph, nc, "mb%d" % i, [128, S_LEN], BF16) for i in range(2)]
        cand = T(ph, nc, "cand", [128, S_LEN], BF16)
        rank = T(ph, nc, "rank", [128, S_LEN], F32)
        hip = T(ph, nc, "hip", [128, 1], F32)
        wt = T(ph, nc, "wt", [128, NITER], F32)
        nhw = T(ph, nc, "nhw", [128, NITER], F32)
        ftab = T(ph, nc, "ftab", [128, NITER], F32)
        chi = T(ph, nc, "chi", [128, 1], F32)
        rr = T(ph, nc, "rr", [128, 1], F32)
        aq = [T(ph, nc, "aq%d" % i, [128, 4, 128], BF16) for i in range(2)]
        iq = [T(ph, nc, "iq%d" % i, [128, 2, 128], BF16) for i in range(2)]
        iw = [T(ph, nc, "iw%d" % i, [128, 4], F32) for i in range(2)]
        rl = [T(ph, nc, "rl%d" % i, [128, 512], F32) for i in range(2)]
        lo = T(ph, nc, "lo", [128, 1], F32)
        hi = T(ph, nc, "hi", [128, 1], F32)
        w0 = T(ph, nc, "w0", [128, 1], F32)
        mid = T(ph, nc, "mid", [128, 1], F32)
        cnt = T(ph, nc, "cnt", [128, 1], F32)
        g2 = T(ph, nc, "g2", [128, 1], F32)
        tmn = T(ph, nc, "tmn", [128, 1], F32)
        E = [[T(ph, nc, "Ea%d%d" % (hg, b), [128, 512], BF16) for b in range(2)] for hg in range(2)]
        rD = T(ph, nc, "rD", [128, 512], F32)
        olat = T(ph, nc, "olat", [128, 2, 512], BF16)
        ao = [T(ph, nc, "ao%d" % i, [128, 4, 128], BF16) for i in range(2)]
        pd = [PS(ph, nc, "pd%d" % i) for i in range(2)]
        pl = [PS(ph, nc, "pla%d" % i) for i in range(2)]
        pO = [PS(ph, nc, "pOa%d" % i) for i in range(2)]
        pD = [PS(ph, nc, "pDa%d" % i) for i in range(2)]
        ident = cst[:, C_ID:C_ID + 128]
        S.add("pool", lambda p_: p_.memset(wuv[:], 0.0), w=[("wuvz",)])
        for h in range(8):
            S.dma("pool", wuv[:, h, (h % 2) * 64:(h % 2) * 64 + 64], wuv_d[h], r=[("wuvz",)], w=[("wuv", h)])
        wuvr = [("wuv", h) for h in range(8)]
        for k in range(4):
            S.add("act", lambda a, k=k: a.mul(out=irep[:, k * 128:(k + 1) * 128], in_=ident, mul=BIGM), r=[("cst",)], w=[("irep", k)])
        irr = [("irep", k) for k in range(4)]
        S.add("dve", lambda v: v.memset(onesb[:], 1.0), w=[("onesb",)])
        for it in range(NITER):
            S.add("pool", lambda p_, it=it: p_.memset(ftab[:, it:it + 1], 0.5 ** (it + 1)), w=[("ftab",)])
        S.dma("sp", akT[:], qkT_d[4], w=[("akT",)])
        S.dma("sp", ikT[:], qkT_d[7], w=[("ikT",)])
        avv_ = av_d.rearrange("(n p) e -> p n e", p=128)
        for n0 in range(0, NT, 8):
            n1 = min(NT, n0 + 8)
            S.dma("sp", vT[:, n0:n1, :], avv_[:, n0:n1, :], w=[("vT", n0)])
        def front(j):
            s = j % 2
            NK = (j + 1) * 128
            js = slice(j * 128, (j + 1) * 128)
            S.dma("sp", aq[s][:], qkT_d[0:4, :, js].rearrange("c p q -> p c q"), w=[("aq", s)])
            S.dma("sp", iq[s][:], qkT_d[5:7, :, js].rearrange("c p q -> p c q"), w=[("iq", s)])
            S.dma("sp", iw[s][:], iw_d[js, :], w=[("iw", s)])
            ntile = (NK + 511) // 512
            sck = [("sc", t) for t in range(ntile)]
            for t in range(ntile):
                w_ = min(512, NK - 512 * t)
                for h in range(4):
                    b = h % 2
                    rs = slice((h % 2) * 64, (h % 2) * 64 + 64)
                    S.add("pe", lambda t_, h=h, b=b, rs=rs, t=t, w_=w_, s=s: t_.matmul(
                        pd[b][:, 0:w_], lhsT=iq[s][rs, h // 2, :], rhs=ikT[rs, t * 512:t * 512 + w_], start=True, stop=True),
                        r=[("iq", s), ("ikT",)], w=[("pd", b)])
                    S.add("act", lambda a, b=b, w_=w_: a.activation(out=rl[b][:, 0:w_], in_=pd[b][:, 0:w_], func=AF.Relu),
                          r=[("pd", b)], w=[("rl", b)])
                    if h == 0:
                        S.add("act", lambda a, b=b, t=t, w_=w_, s=s: a.activation(
                            out=sc[:, t * 512:t * 512 + w_], in_=rl[b][:, 0:w_], func=AF.Copy, scale=iw[s][:, 0:1]),
                            r=[("rl", b), ("iw", s)], w=[("sc", t)])
                    else:
                        S.add("dve", lambda v, b=b, t=t, w_=w_, s=s, h=h: v.scalar_tensor_tensor(
                            out=sc[:, t * 512:t * 512 + w_], in0=rl[b][:, 0:w_], scalar=iw[s][:, h:h + 1],
                            in1=sc[:, t * 512:t * 512 + w_], op0=ALU.mult, op1=ALU.add),
                            r=[("rl", b), ("iw", s), ("sc", t)], w=[("sc", t)])
            S.add("dve", lambda v, NK=NK: v.memset(sc[0:64, NK - 64:NK], -1.0e30), r=[("sc", ntile - 1)], w=[("sc", ntile - 1)])
            S.add("dve", lambda v, NK=NK: v.reduce_max(out=hi[:], in_=sc[:, 0:NK], axis=AX.X), r=sck, w=[("hi",)])
            S.add("dve", lambda v, NK=NK: v.tensor_reduce(out=lo[:], in_=sc[:, 0:NK - 64], axis=AX.X, op=ALU.min), r=sck, w=[("lo",)])
            S.add("dve", lambda v, NK=NK: v.tensor_reduce(out=tmn[64:128, :], in_=sc[64:128, NK - 64:NK], axis=AX.X, op=ALU.min),
                  r=sck, w=[("tmn",)])
            S.add("dve", lambda v: v.tensor_tensor(out=lo[64:128, :], in0=lo[64:128, :], in1=tmn[64:128, :], op=ALU.min),
                  r=[("lo",), ("tmn",)], w=[("lo",)])
            S.add("dve", lambda v: v.tensor_scalar(out=lo[:], in0=lo[:], scalar1=-1.0, scalar2=None, op0=ALU.add),
                  r=[("lo",)], w=[("lo",)])
            S.add("dve", lambda v: v.tensor_tensor(out=w0[:], in0=hi[:], in1=lo[:], op=ALU.subtract),
                  r=[("lo",), ("hi",)], w=[("w0",)])
            S.add("dve", lambda v: v.tensor_scalar(out=wt[:], in0=ftab[:], scalar1=w0[:, 0:1], scalar2=None, op0=ALU.mult),
                  r=[("w0",), ("ftab",)], w=[("wt",)])
            S.add("dve", lambda v: v.tensor_scalar(out=nhw[:], in0=wt[:], scalar1=-0.5, scalar2=None, op0=ALU.mult),
                  r=[("wt",)], w=[("nhw",)])
            S.add("dve", lambda v: v.tensor_tensor(out=mid[:], in0=lo[:], in1=wt[:, 0:1], op=ALU.add),
                  r=[("lo",), ("wt",)], w=[("mid",)])
            for it in range(NITER):
                S.add("dve", lambda v, NK=NK: v.tensor_scalar(out=mb[s][:, 0:NK], in0=sc[:, 0:NK], scalar1=mid[:, 0:1], scalar2=None,
                                                              op0=ALU.is_gt, op1=ALU.add, accum_out=cnt[:]),
                      r=sck + [("mid",)], w=[("mb", s), ("cnt",)])
                S.add("dve", lambda v, it=it: v.tensor_scalar(out=g2[:], in0=cnt[:], scalar1=TOPK, scalar2=wt[:, it:it + 1],
                                                              op0=ALU.is_ge, op1=ALU.mult),
                      r=[("cnt",), ("wt",)], w=[("g2",)])
                S.add("dve", lambda v, it=it: v.scalar_tensor_tensor(out=mid[:], in0=g2[:], scalar=nhw[:, it:it + 1], in1=mid[:],
                                                                     op0=ALU.add, op1=ALU.add),
                      r=[("g2",), ("nhw",), ("mid",)], w=[("mid",)])
            fe = 0.5 ** (NITER + 1)
            S.add("dve", lambda v: v.scalar_tensor_tensor(out=lo[:], in0=w0[:], scalar=-fe, in1=mid[:], op0=ALU.mult, op1=ALU.add),
                  r=[("w0",), ("mid",)], w=[("lo",)])
            S.add("dve", lambda v: v.scalar_tensor_tensor(out=hip[:], in0=w0[:], scalar=fe, in1=mid[:], op0=ALU.mult, op1=ALU.add),
                  r=[("w0",), ("mid",)], w=[("hip",)])
            S.add("dve", lambda v, NK=NK: v.tensor_scalar(out=mb[s][:, 0:NK], in0=sc[:, 0:NK], scalar1=hip[:, 0:1], scalar2=None,
                                                          op0=ALU.is_gt, op1=ALU.add, accum_out=chi[:]),
                  r=sck + [("hip",)], w=[("mb", s), ("chi",)])
            S.add("dve", lambda v: v.tensor_scalar(out=rr[:], in0=chi[:], scalar1=-1.0, scalar2=TOPK, op0=ALU.mult, op1=ALU.add),
                  r=[("chi",)], w=[("rr",)])
            S.add("dve", lambda v, NK=NK: v.scalar_tensor_tensor(out=cand[:, 0:NK], in0=sc[:, 0:NK], scalar=lo[:, 0:1], in1=mb[s][:, 0:NK],
                                                                 op0=ALU.is_gt, op1=ALU.subtract),
                  r=sck + [("lo",), ("mb", s)], w=[("cand",)])
            S.add("dve", lambda v, NK=NK: v.tensor_tensor_scan(out=rank[:, 0:NK], data0=cand[:, 0:NK], data1=cand[:, 0:NK], initial=0.0,
                                                               op0=ALU.add, op1=ALU.max),
                  r=[("cand",)], w=[("rank",)])
            S.add("dve", lambda v, NK=NK: v.scalar_tensor_tensor(out=cand[:, 0:NK], in0=rank[:, 0:NK], scalar=rr[:, 0:1], in1=cand[:, 0:NK],
                                                                 op0=ALU.is_le, op1=ALU.mult),
                  r=[("rank",), ("rr",), ("cand",)], w=[("cand",)])
            S.add("dve", lambda v, NK=NK: v.scalar_tensor_tensor(out=mb[s][:, 0:NK], in0=mb[s][:, 0:NK], scalar=-1.0, in1=cand[:, 0:NK],
                                                                 op0=ALU.add, op1=ALU.add),
                  r=[("mb", s), ("cand",)], w=[("mb", s)])
        def back(j):
            s = j % 2
            js = slice(j * 128, (j + 1) * 128)
            for kt in range(j + 1):
                b = kt % 2
                for hg in range(2):
                    rs = slice(hg * 64, hg * 64 + 64)
                    def lg(t_, hg=hg, rs=rs, kt=kt, s=s):
                        t_.matmul(pl[hg][:], lhsT=akT[rs, kt * 128:(kt + 1) * 128],
                                  rhs=aq[s][rs, :, :].rearrange("p c q -> p (c q)"), start=True, stop=False)
                        return t_.matmul(pl[hg][:], lhsT=mb[s][:, kt * 128:(kt + 1) * 128], rhs=irep[:], start=False, stop=True)
                    S.add("pe", lg, r=[("akT",), ("aq", s), ("mb", s)] + irr, w=[("pl", hg)])
                    S.add("act", lambda a, hg=hg, b=b: a.activation(out=E[hg][b][:], in_=pl[hg][:], func=AF.Exp, scale=0.125),
                          r=[("pl", hg)], w=[("E", hg, b)])
                for hg in range(2):
                    def pv(t_, hg=hg, kt=kt, b=b, j=j):
                        t_.matmul(pO[hg][:], lhsT=vT[:, kt, :], rhs=E[hg][b][:], start=(kt == 0), stop=(kt == j))
                        return t_.matmul(pD[hg][:], lhsT=onesb[:], rhs=E[hg][b][:], start=(kt == 0), stop=(kt == j))
                    S.add("pe", pv, r=[("E", hg, b), ("vT", (kt // 8) * 8), ("onesb",)], w=[("pO", hg), ("pD", hg)])
            for hg in range(2):
                S.add("dve", lambda v, hg=hg: v.reciprocal(out=rD[:], in_=pD[hg][:]), r=[("pD", hg)], w=[("rD",)])
                S.add("dve", lambda v, hg=hg: v.tensor_tensor(out=olat[:, hg, :], in0=pO[hg][:], in1=rD[:], op=ALU.mult),
                      r=[("pO", hg), ("rD",)], w=[("olat", hg)])
            def uv(t_):
                for i in range(4):
                    t_.matmul(pd[0][:, i * 128:(i + 1) * 128], lhsT=wuv[:, 2 * i, :], rhs=olat[:, 0, i * 128:(i + 1) * 128],
                              start=True, stop=False)
                    ins = t_.matmul(pd[0][:, i * 128:(i + 1) * 128], lhsT=wuv[:, 2 * i + 1, :], rhs=olat[:, 1, i * 128:(i + 1) * 128],
                                    start=False, stop=True)
                return ins
            S.add("pe", uv, r=[("olat", 0), ("olat", 1)] + wuvr, w=[("pd", 0)])
            S.add("act", lambda a, s=s: a.copy(out=ao[s][:].rearrange("p c q -> p (c q)"), in_=pd[0][:]), r=[("pd", 0)], w=[("ao", s)])
            S.dma("pool", catT_d[0:4, :, js].rearrange("c p q -> p c q"), ao[s][:], r=[("ao", s)], w=[("catd", j)])
        front(0)
        for j in range(NT):
            if j + 1 < NT:
                front(j + 1)
            back(j)
        S.emit()


def phase_odd(S, nc, cst, gcol, hT_d, win_d, wout_d, oddc_d, oddb_d, cws_d, cbs_d, NG):
    with ExitStack() as ph:
        Win = T(ph, nc, "Win", [128, 8, 2048], BF16)
        Wout = T(ph, nc, "Wout", [128, 8, D], BF16)
        diag = T(ph, nc, "diag", [128, 4, 31, 128], BF16)
        WsT = T(ph, nc, "WsT", [128, 8, 128], BF16)
        cbs = T(ph, nc, "cbs", [8, 128], F32)
        oddc = T(ph, nc, "oddc", [128, 16 + 4 * 31], F32)
        oddb = T(ph, nc, "oddb", [128, 2, 4, 2, 64], F32)
        wsl = [T(ph, nc, "wsl%d" % i, [128, 128], F32) for i in range(2)]
        hb = [T(ph, nc, "hb%d" % i, [128, 8, TG], F32) for i in range(2)]
        xn = T(ph, nc, "xn", [128, 8, TG], BF16)
        sd = T(ph, nc, "sd", [128, TG], F32)
        sg = [T(ph, nc, "sg%d" % i, [128, TG], F32) for i in range(2)]
        uT = T(ph, nc, "uT", [128, 4, TG], BF16)
        cat = T(ph, nc, "cat", [128, 8, TG], BF16)
        vt = [T(ph, nc, "vt%d" % i, [128, 4, 2, 64], F32) for i in range(2)]
        vnp = [T(ph, nc, "vnp%d" % i, [128, 4, 2, 128], BF16) for i in range(2)]
        st = T(ph, nc, "st", [128, 6], F32)
        mv = T(ph, nc, "mv", [128, 2], F32)
        sdv = T(ph, nc, "sdv", [128, 1], F32)
        hdb = T(ph, nc, "hdb", [128, 4, 30 + TG], BF16)
        y = T(ph, nc, "y", [128, 4, TG], F32)
        mu = T(ph, nc, "mu", [128, TG], F32)
        rs2 = T(ph, nc, "rs2", [128, TG], F32)
        tmp = [T(ph, nc, "tmp%d" % i, [128, TG], F32) for i in range(2)]
        pst = PS(ph, nc, "pst")
        pz = [PS(ph, nc, "pz%d" % i) for i in range(2)]
        pg = [PS(ph, nc, "pg%d" % i) for i in range(2)]
        pvt = PS(ph, nc, "pvt")
        psg = PS(ph, nc, "psg")
        pc = PS(ph, nc, "pc")
        ident = cst[:, C_ID:C_ID + 128]
        o512 = cst[:, C_O512:C_O512 + 128]
        for c in range(8):
            S.dma("pool", Win[:, c, :], win_d[c * 128:(c + 1) * 128, :], w=[("Win", c)])
        for c in range(8):
            S.dma("pool", Wout[:, c, :], wout_d[c * 128:(c + 1) * 128, :], w=[("Wout", c)])
        wir = [("Win", c) for c in range(8)]
        wor = [("Wout", c) for c in range(8)]
        S.dma("sp", oddc[:], oddc_d, w=[("oddc",)])
        S.dma("sp", oddb[:].rearrange("p a g t c -> p a (g t c)"), oddb_d, w=[("oddb",)])
        S.dma("sp", cbs[:], cbs_d, w=[("cbs",)])
        for ch in range(4):
            for k in range(31):
                eng = "dve" if (ch * 31 + k) % 2 == 0 else "pool"
                S.add(eng, lambda e, ch=ch, k=k: e.tensor_scalar(out=diag[:, ch, k, :], in0=ident,
                                                                 scalar1=oddc[:, 16 + ch * 31 + k:16 + ch * 31 + k + 1],
                                                                 scalar2=None, op0=ALU.mult),
                      r=[("oddc",), ("cst",)], w=[("diag", ch, k)])
        for g_ in range(8):
            b = g_ % 2
            S.dma("sp", wsl[b][:], cws_d[g_], w=[("wsl", b)])
            S.add("dve", lambda v, b=b: v.tensor_tensor(out=wsl[b][:], in0=wsl[b][:], in1=cst[:, C_SGM:C_SGM + 128], op=ALU.mult),
                  r=[("wsl", b)], w=[("wsl", b)])
            S.add("pe", lambda t, b=b: t.transpose(out=pvt[:, 0:128], in_=wsl[b][:], identity=ident), r=[("wsl", b)], w=[("pvt",)])
            S.add("act", lambda a, g_=g_: a.copy(out=WsT[:, g_, :], in_=pvt[:, 0:128]), r=[("pvt",)], w=[("WsT", g_)])
        wsr = [("WsT", g_) for g_ in range(8)]
        for i in range(2):
            S.add("pool", lambda p_, i=i: p_.memset(vnp[i][:], 0.0), w=[("vnp", i)])
        S.add("pool", lambda p_: p_.memset(hdb[:], 0.0), w=[("hdb", ch) for ch in range(4)])
        S.dma("sp", hb[0][:], hT_d[:, :, 0:TG], w=[("h", 0, c) for c in range(8)])
        for g in range(NG):
            sl = g % 2
            if g + 1 < NG:
                S.dma("sp", hb[1 - sl][:], hT_d[:, :, (g + 1) * TG:(g + 2) * TG], w=[("h", 1 - sl, c) for c in range(8)])
            emit_rms(S, cst, hb[sl], ("h", sl), gcol, xn, ("xn",), sg, sd, sd, pst, "o")
            xr = [("xn", c) for c in range(8)]
            for ci in range(4):
                b = ci % 2
                def mmu(t, ci=ci, b=b):
                    for c in range(8):
                        ins = t.matmul(pz[b][:], lhsT=Win[:, c, ci * 128:(ci + 1) * 128], rhs=xn[:, c, :], start=(c == 0), stop=(c == 7))
                    return ins
                S.add("pe", mmu, r=xr + wir, w=[("pz", b)])
                S.add("act", lambda a, ci=ci, b=b: a.activation(out=uT[:, ci, :], in_=pz[b][:], func=AF.Gelu), r=[("pz", b)], w=[("uT", ci)])
            def v_pre(s4):
                b = s4 % 2
                ts_ = slice(s4 * 128, (s4 + 1) * 128)
                def mmv(t, ts_=ts_):
                    for c in range(8):
                        ins = t.matmul(pvt[:], lhsT=xn[:, c, ts_], rhs=Win[:, c, 512:1024], start=(c == 0), stop=(c == 7))
                    return ins
                S.add("pe", mmv, r=xr + wir, w=[("pvt",)])
                vflat = vt[b][:].rearrange("p g t c -> p (g t c)")
                S.add("act", lambda a, vflat=vflat: a.activation(out=vflat, in_=pvt[:], func=AF.Gelu), r=[("pvt",)], w=[("vt", b)])
                S.add("dve", lambda v, vflat=vflat: v.bn_stats(out=st[:], in_=vflat), r=[("vt", b)], w=[("st",)])
                S.add("dve", lambda v: v.bn_aggr(out=mv[:], in_=st[:]), r=[("st",)], w=[("mv",)])
                S.add("act", lambda a: a.activation(out=sdv[:], in_=mv[:, 1:2], func=AF.Sqrt, bias=EPS, scale=1.0), r=[("mv",)], w=[("sdv",)])
                S.add("dve", lambda v: v.reciprocal(out=sdv[:], in_=sdv[:]), r=[("sdv",)], w=[("sdv",)])
                S.add("dve", lambda v, vflat=vflat: v.tensor_scalar(out=vflat, in0=vflat, scalar1=mv[:, 0:1], scalar2=sdv[:, 0:1],
                                                                    op0=ALU.subtract, op1=ALU.mult),
                      r=[("vt", b), ("mv",), ("sdv",)], w=[("vt", b)])
                S.add("dve", lambda v, b=b: v.tensor_tensor(out=vt[b][:], in0=vt[b][:], in1=oddb[:, 0], op=ALU.mult),
                      r=[("vt", b), ("oddb",)], w=[("vt", b)])
                S.add("dve", lambda v, b=b: v.tensor_tensor(out=vnp[b][:, :, 0, 0:64], in0=vt[b][:, :, 0, :], in1=oddb[:, 1, :, 0, :], op=ALU.add),
                      r=[("vt", b), ("oddb",)], w=[("vnp", b)])
                S.add("dve", lambda v, b=b: v.tensor_tensor(out=vnp[b][:, :, 1, 64:128], in0=vt[b][:, :, 1, :], in1=oddb[:, 1, :, 1, :], op=ALU.add),
                      r=[("vt", b), ("oddb",)], w=[("vnp", b)])
            def v_post(s4):
                b = s4 % 2
                ts_ = slice(s4 * 128, (s4 + 1) * 128)
                def sgu(t, b=b):
                    for gp in range(4):
                        o_ = psg[:, gp * 128:(gp + 1) * 128]
                        t.matmul(o_, lhsT=vnp[b][:, gp, 0, :], rhs=WsT[:, 2 * gp, :], start=True, stop=False)
                        t.matmul(o_, lhsT=vnp[b][:, gp, 1, :], rhs=WsT[:, 2 * gp + 1, :], start=False, stop=False)
                        ins = t.matmul(o_, lhsT=cst[0:8, C_SELB + gp * 128:C_SELB + (gp + 1) * 128], rhs=cbs[:, :], start=False, stop=True)
                    return ins
                S.add("pe", sgu, r=[("vnp", b), ("cbs",), ("cst",)] + wsr, w=[("psg",)])
                S.add("dve", lambda v, ts_=ts_: v.tensor_tensor(out=cat[:, 0:4, ts_], in0=psg[:].rearrange("p (g i) -> p g i", g=4),
                                                                in1=uT[:, :, ts_], op=ALU.mult),
                      r=[("psg",)] + [("uT", ci) for ci in range(4)], w=[("cat", s4)])
            def conv_part(ch):
                b = ch % 2
                def mma(t, ch=ch, b=b):
                    for c in range(8):
                        ins = t.matmul(pz[b][:], lhsT=Win[:, c, 1024 + ch * 128:1024 + (ch + 1) * 128], rhs=xn[:, c, :],
                                       start=(c == 0), stop=(c == 7))
                    return ins
                def mmg(t, ch=ch, b=b):
                    for c in range(8):
                        ins = t.matmul(pg[b][:], lhsT=Win[:, c, 1536 + ch * 128:1536 + (ch + 1) * 128], rhs=xn[:, c, :],
                                       start=(c == 0), stop=(c == 7))
                    return ins
                S.add("pe", mma, r=xr + wir, w=[("pz", b)])
                S.add("pe", mmg, r=xr + wir, w=[("pg", b)])
                S.add("act", lambda a, b=b: a.activation(out=sg[b][:], in_=pg[b][:], func=AF.Sigmoid), r=[("pg", b)], w=[("sg", b)])
                S.add("dve", lambda v, ch=ch, b=b: v.tensor_tensor(out=hdb[:, ch, 30:30 + TG], in0=pz[b][:], in1=sg[b][:], op=ALU.mult),
                      r=[("pz", b), ("sg", b)], w=[("hdb", ch)])
                def conv(t, ch=ch):
                    for k in range(31):
                        ins = t.matmul(pc[:], lhsT=diag[:, ch, k, :], rhs=hdb[:, ch, k:k + TG], start=(k == 0), stop=(k == 30))
                    return ins
                S.add("pe", conv, r=[("hdb", ch)] + [("diag", ch, k) for k in range(31)], w=[("pc",)])
                S.add("act", lambda a, ch=ch: a.activation(out=y[:, ch, :], in_=pc[:], func=AF.Identity, bias=oddc[:, ch:ch + 1], scale=1.0),
                      r=[("pc",), ("oddc",)], w=[("y", ch)])
                S.add("pool", lambda p_, ch=ch: p_.tensor_copy(out=hdb[:, ch, 0:30], in_=hdb[:, ch, TG:TG + 30]),
                      r=[("hdb", ch)], w=[("hdb", ch)])
            for i4 in range(4):
                v_pre(i4)
                conv_part(i4)
                v_post(i4)
            for ch in range(4):
                b = ch % 2
                S.add("pe", lambda t, ch=ch: t.matmul(pst[:], lhsT=o512, rhs=y[:, ch, :], start=(ch == 0), stop=(ch == 3)),
                      r=[("y", ch)], w=[("pst",)])
                S.add("act", lambda a, ch=ch, b=b: a.activation(out=tmp[b][:], in_=y[:, ch, :], func=AF.Square), r=[("y", ch)], w=[("tmp", b)])
                S.add("pe", lambda t, ch=ch, b=b: t.matmul(pvt[:], lhsT=o512, rhs=tmp[b][:], start=(ch == 0), stop=(ch == 3)),
                      r=[("tmp", b)], w=[("pvt",)])
            S.add("act", lambda a: a.copy(out=mu[:], in_=pst[:]), r=[("pst",)], w=[("mu",)])
            S.add("dve", lambda v: v.tensor_tensor(out=rs2[:], in0=mu[:], in1=mu[:], op=ALU.mult), r=[("mu",)], w=[("rs2",)])
            S.add("dve", lambda v: v.tensor_tensor(out=rs2[:], in0=pvt[:], in1=rs2[:], op=ALU.subtract), r=[("pvt",), ("rs2",)], w=[("rs2",)])
            S.add("act", lambda a: a.activation(out=rs2[:], in_=rs2[:], func=AF.Sqrt, bias=EPS, scale=1.0), r=[("rs2",)], w=[("rs2",)])
            S.add("dve", lambda v: v.reciprocal(out=rs2[:], in_=rs2[:]), r=[("rs2",)], w=[("rs2",)])
            for ch in range(4):
                b = ch % 2
                S.add("dve", lambda v, ch=ch, b=b: v.tensor_tensor(out=tmp[b][:], in0=y[:, ch, :], in1=mu[:], op=ALU.subtract),
                      r=[("y", ch), ("mu",)], w=[("tmp", b)])
                S.add("dve", lambda v, b=b: v.tensor_tensor(out=tmp[b][:], in0=tmp[b][:], in1=rs2[:], op=ALU.mult),
                      r=[("tmp", b), ("rs2",)], w=[("tmp", b)])
                S.add("act", lambda a, ch=ch, b=b: a.activation(out=cat[:, 4 + ch, :], in_=tmp[b][:], func=AF.Silu,
                                                                bias=oddc[:, 8 + ch:9 + ch], scale=oddc[:, 4 + ch:5 + ch]),
                      r=[("tmp", b), ("oddc",)], w=[("catd", ch)])
            catr = [("cat", s4) for s4 in range(4)] + [("catd", ch) for ch in range(4)]
            for m in range(8):
                b = m % 2
                def mmo(t, m=m, b=b):
                    for c in range(8):
                        ins = t.matmul(pz[b][:], lhsT=Wout[:, c, m * 128:(m + 1) * 128], rhs=cat[:, c, :], start=(c == 0), stop=(c == 7))
                    return ins
                S.add("pe", mmo, r=catr + wor, w=[("pz", b)])
                S.add("dve", lambda v, m=m, b=b, sl=sl: v.tensor_tensor(out=hb[sl][:, m, :], in0=pz[b][:], in1=hb[sl][:, m, :], op=ALU.add),
                      r=[("pz", b), ("h", sl, m)], w=[("h", sl, m)])
            S.dma("pool", hT_d[:, :, g * TG:(g + 1) * TG], hb[sl][:], r=[("h", sl, c) for c in range(8)], w=[("hTd", g)])
        S.emit()

ALL_STAGES = ("tin", "ffn1_0", "evenproj", "dsa", "diff", "wout0", "ffn2_0", "ple0",
              "ffn1_1", "odd", "ffn2_1", "ple1")


def build(S_LEN, stages=ALL_STAGES, dbg=False):
    NG = S_LEN // TG
    nc = bass.Bass("TRN2", target_bir_lowering=False)

    def din(name, shape, dt=F32):
        return nc.dram_tensor(name, shape, dt, kind="ExternalInput").ap()

    def dscr(name, shape, dt):
        return nc.dram_tensor(name, shape, dt, kind="ExternalOutput" if dbg else "Internal").ap()

    x_d = din("x", [S_LEN, D])
    p_d = din("p", [2, S_LEN, 256])
    posb_d = din("posb", [128, S_LEN], I32)
    cst_d = din("consts", [128, NCONST])
    gcols_d = din("gcols", [128, 64])
    hgc_d = din("hgc", [128, 8])
    ffw = {}
    for nm, shp in (("ffn1_wg", [2, D, DFF]), ("ffn1_wu", [2, D, DFF]), ("ffn1_wd", [2, DFF, D]),
                    ("ffn2_wg", [2, D, DFF]), ("ffn2_wu", [2, D, DFF]), ("ffn2_wd", [2, DFF, D])):
        ffw[nm] = din(nm, shp)
    wfm_d = din("w_in0_fm", [D, 2048])
    wtm_d = din("w_in0_tm", [D, 644])
    ev_w_out_d = din("ev_w_out", [1, D, D])
    ple_wgate_d = din("ple_wgate", [2, D, D])
    ple_wproj_d = din("ple_wproj", [2, 256, D])
    a_w_uv_d = din("a_w_uv", [1, 8, 128, 64])
    lamv_d = din("lamv", [128, 4, 64])
    od_w_in_d = din("od_w_in", [1, D, 2048])
    od_w_out_d = din("od_w_out", [1, D, D])
    oddc_d = din("oddc", [128, 16 + 4 * 31])
    oddb_d = din("oddb", [128, 2, 512])
    c_w_s_d = din("c_w_s", [1, 8, 128, 128])
    c_b_s_d = din("c_b_s", [1, 8, 128])
    out_d = nc.dram_tensor("out", [S_LEN, D], F32, kind="ExternalOutput").ap()
    hT_d = dscr("hT", [128, 8, S_LEN], F32)
    qkT_d = dscr("qkT", [16, 128, S_LEN], BF16)
    bv_d = dscr("bv", [S_LEN, 512], BF16)
    av_d = dscr("av", [S_LEN, 128], BF16)
    iw_d = dscr("iw", [S_LEN, 4], F32)
    catT_d = dscr("catT", [8, 128, S_LEN], BF16)
    with ExitStack() as es:
        S = Sched(nc, es)
        cst = T(es, nc, "cst", [128, NCONST], F32)
        gcols = T(es, nc, "gcols_sb", [128, 64], F32)
        hgc = T(es, nc, "hgc_sb", [128, 8], F32)
        S.dma("sp", cst[:], cst_d, w=[("cst",)])
        S.dma("sp", gcols[:], gcols_d, w=[("gcols",)])
        S.dma("sp", hgc[:], hgc_d, w=[("hgc",)])
        S.emit()

        def gc(layer, which):
            o = (layer * 4 + which) * 8
            return gcols[:, o:o + 8]
        if "tin" in stages:
            phase_transpose_in(S, nc, cst, x_d, hT_d, NG)
        if "ffn1_0" in stages:
            phase_ffn(S, nc, cst, gc(0, 0), hT_d, ffw["ffn1_wg"][0], ffw["ffn1_wu"][0], ffw["ffn1_wd"][0], NG)
        if "evenproj" in stages:
            phase_evenproj(S, nc, cst, gc(0, 1), hgc, hT_d, posb_d, wfm_d, wtm_d, qkT_d, bv_d, av_d, iw_d, NG)
        if "dsa" in stages:
            phase_dsa(S, nc, cst, qkT_d, av_d, iw_d, a_w_uv_d[0], catT_d, S_LEN)
        if "diff" in stages:
            phase_diff(S, nc, cst, hgc, lamv_d, qkT_d, bv_d, catT_d, S_LEN)
        if "wout0" in stages:
            phase_proj_res(S, nc, hT_d, catT_d, ev_w_out_d[0], NG)
        if "ffn2_0" in stages:
            phase_ffn(S, nc, cst, gc(0, 2), hT_d, ffw["ffn2_wg"][0], ffw["ffn2_wu"][0], ffw["ffn2_wd"][0], NG)
        if "ple0" in stages:
            phase_ple(S, nc, cst, gc(0, 3), hT_d, p_d[0], ple_wgate_d[0], ple_wproj_d[0], None, NG)
        if "ffn1_1" in stages:
            phase_ffn(S, nc, cst, gc(1, 0), hT_d, ffw["ffn1_wg"][1], ffw["ffn1_wu"][1], ffw["ffn1_wd"][1], NG)
        if "odd" in stages:
            phase_odd(S, nc, cst, gc(1, 1), hT_d, od_w_in_d[0], od_w_out_d[0], oddc_d, oddb_d, c_w_s_d[0], c_b_s_d[0], NG)
        if "ffn2_1" in stages:
            phase_ffn(S, nc, cst, gc(1, 2), hT_d, ffw["ffn2_wg"][1], ffw["ffn2_wu"][1], ffw["ffn2_wd"][1], NG)
        if "ple1" in stages:
            phase_ple(S, nc, cst, gc(1, 3), hT_d, p_d[1], ple_wgate_d[1], ple_wproj_d[1], out_d, NG)
    return nc


def shared_inputs(inputs):
    f = lambda k: np.asarray(inputs[k], np.float32)
    m = {}
    m["consts"] = make_consts()
    g = np.zeros((128, 64), np.float32)
    for layer in range(2):
        for wi, nm in enumerate(("ffn1_g", "mix_g", "ffn2_g", "ple_g")):
            o = (layer * 4 + wi) * 8
            g[:, o:o + 8] = f(nm)[layer].reshape(8, 128).T
    m["gcols"] = g
    hg = np.zeros((128, 8), np.float32)
    for i, nm in enumerate(("a_q_g", "a_k_g", "b_q_g", "b_k_g")):
        hg[:, i] = np.tile(f(nm)[0], 2)
    hg[:, 4] = f("b_subln_g")[0]
    m["hgc"] = hg
    for nm in ("ffn1_wg", "ffn1_wu", "ffn1_wd", "ffn2_wg", "ffn2_wu", "ffn2_wd", "ev_w_out", "ple_wgate",
               "ple_wproj", "a_w_uv", "od_w_in", "od_w_out", "c_w_s", "c_b_s"):
        m[nm] = f(nm)
    w = f("ev_w_in")[0]
    aq, ak, av, iq, ik, iw, bq, bk, bv = (w[:, 0:512], w[:, 512:576], w[:, 576:704], w[:, 704:960], w[:, 960:1024],
                                          w[:, 1024:1028], w[:, 1028:1540], w[:, 1540:2052], w[:, 2052:2564])
    m["w_in0_fm"] = np.ascontiguousarray(np.concatenate([aq, ak, ak, iq, ik, ik, bq, bk], axis=1))
    m["w_in0_tm"] = np.ascontiguousarray(np.concatenate([bv, av, iw], axis=1))
    lam = np.stack([f("b_lam_q1")[0], f("b_lam_k1")[0], f("b_lam_q2")[0], f("b_lam_k2")[0]], axis=0)
    m["lamv"] = np.ascontiguousarray(np.broadcast_to(lam[None], (128, 4, 64)))
    oc = np.zeros((128, 16 + 4 * 31), np.float32)
    for i, nm in enumerate(("d_conv_b", "d_ln_g", "d_ln_b")):
        oc[:, i * 4:(i + 1) * 4] = f(nm)[0].reshape(4, 128).T
    oc[:, 16:] = f("d_conv_w")[0].T.reshape(4, 128, 31).transpose(1, 0, 2).reshape(128, 4 * 31)
    m["oddc"] = oc
    m["oddb"] = np.ascontiguousarray(np.broadcast_to(np.stack([f("c_ln_g")[0], f("c_ln_b")[0]], 0)[None], (128, 2, 512)))
    return m


def host_inputs(inputs, b, S_LEN, shared=None):
    m = dict(shared if shared is not None else shared_inputs(inputs))
    m["x"] = np.ascontiguousarray(np.asarray(inputs["x"])[b, :S_LEN])
    m["p"] = np.ascontiguousarray(np.asarray(inputs["p"])[:, b, :S_LEN])
    m["posb"] = np.ascontiguousarray(np.broadcast_to(np.asarray(inputs["pos"])[b, :S_LEN][None], (128, S_LEN))).astype(np.int32)
    return m


_NC_CACHE = {}


def kernel(**inputs):
    S_LEN = 8192
    if S_LEN not in _NC_CACHE:
        _NC_CACHE[S_LEN] = build(S_LEN)
    nc = _NC_CACHE[S_LEN]
    sh = shared_inputs(inputs)
    in_maps = [host_inputs(inputs, b, S_LEN, sh) for b in range(8)]
    res = run_bass_kernel_spmd(nc, in_maps, core_ids=list(range(8)))
    return np.stack([np.asarray(res.results[b]["out"], np.float32) for b in range(8)], axis=0)
```

```python
from contextlib import ExitStack
import math
import numpy as np
import concourse.bass as bass
import concourse.mybir as mybir
from concourse.bass_utils import run_bass_kernel_spmd

F32 = mybir.dt.float32
BF16 = mybir.dt.bfloat16
I32 = mybir.dt.int32
AF = mybir.ActivationFunctionType
ALU = mybir.AluOpType
AX = mybir.AxisListType

D = 1024
DFF = 2816
NF = DFF // 128
TG = 512
EPS = 1e-6
ENGS = ("pe", "act", "dve", "pool", "sp")
NDMA = 12


class Op:
    __slots__ = ("eng", "fn", "deps", "dma", "sig", "has_dep", "idx")

    def __init__(self, eng, fn, dma):
        self.eng = eng
        self.fn = fn
        self.dma = dma
        self.deps = []
        self.sig = None
        self.has_dep = False


class Sched:
    def __init__(self, nc, es):
        self.nc = nc
        self.cnt = {e: 0 for e in ENGS}
        self.sem = {e: es.enter_context(nc.semaphore("s_" + e)) for e in ENGS}
        self.dsem = {q: [es.enter_context(nc.semaphore("d_%s%d" % (q, i))) for i in range(NDMA)]
                     for q in ("sp", "act", "pool")}
        self.dcnt = {q: 0 for q in ("sp", "act", "pool")}
        self.waited = {}
        self.reset_phase()

    def reset_phase(self):
        self.ops = []
        self.lastw = {}
        self.readers = {}

    def add(self, eng, fn, r=(), w=(), dma=False):
        op = Op(eng, fn, dma)
        op.idx = len(self.ops)
        deps = set()
        for k in r:
            j = self.lastw.get(k)
            if j is not None:
                deps.add(j)
        for k in w:
            j = self.lastw.get(k)
            if j is not None:
                deps.add(j)
            for j in self.readers.get(k, ()):
                deps.add(j)
        for k in r:
            self.readers.setdefault(k, []).append(op.idx)
        for k in w:
            self.lastw[k] = op.idx
            self.readers[k] = []
        deps.discard(op.idx)
        for j in sorted(deps):
            d = self.ops[j]
            if d.eng == "pe" and eng == "pe" and not d.dma and not dma:
                continue
            op.deps.append(j)
            d.has_dep = True
        self.ops.append(op)
        return op

    def dma(self, q, out, in_, r=(), w=(), **kw):
        def fn(e):
            return e.dma_start(out=out, in_=in_, **kw)
        return self.add(q, fn, r, w, dma=True)

    def emit(self):
        nc = self.nc
        ops = self.ops
        last_of = {}
        for op in ops:
            last_of[op.eng] = op
        for op in last_of.values():
            op.has_dep = True
        for op in ops:
            if op.dma:
                q = op.eng
                k = self.dcnt[q]
                self.dcnt[q] += 1
                op.sig = (self.dsem[q][k % NDMA], 16 * (k // NDMA + 1), 16 * (k // NDMA))
            elif op.has_dep:
                self.cnt[op.eng] += 1
                op.sig = (self.sem[op.eng], self.cnt[op.eng], None)
        final = []
        for e in ENGS:
            if self.cnt[e] > 0:
                final.append((self.sem[e], self.cnt[e]))
        for q in self.dsem:
            n = self.dcnt[q]
            for i, s in enumerate(self.dsem[q]):
                if n > i:
                    final.append((s, 16 * ((n - i + NDMA - 1) // NDMA)))
        waited = self.waited

        def wait(e, eobj, sem, val):
            key = (e, id(sem))
            if waited.get(key, 0) >= val:
                return
            waited[key] = val
            eobj.wait_ge(sem, val)

        def run(e):
            def body(eobj):
                for op in ops:
                    if op.eng != e:
                        continue
                    if op.dma and op.sig[2] > 0:
                        wait(e, eobj, op.sig[0], op.sig[2])
                    for j in op.deps:
                        s = ops[j].sig
                        wait(e, eobj, s[0], s[1])
                    ins = op.fn(eobj)
                    if op.sig is not None:
                        ins.then_inc(op.sig[0], 16 if op.dma else 1)
                for (s, v) in final:
                    wait(e, eobj, s, v)
            return body

        with nc.Block() as block:
            block.tensor(run("pe"))
            block.scalar(run("act"))
            block.vector(run("dve"))
            block.gpsimd(run("pool"))
            block.sync(run("sp"))
        self.reset_phase()


C_ID = 0
C_O1024 = 128
C_O512 = 256
C_O128 = 384
C_BD64 = 512
C_ROT = 640
C_ONE = 768
C_SGM = 896
C_INV = 1024
C_SELB = 1028
NCONST = 1028 + 512


def make_consts():
    c = np.zeros((128, NCONST), np.float32)
    c[:, C_ID:C_ID + 128] = np.eye(128, dtype=np.float32)
    c[:, C_O1024:C_O1024 + 128] = 1.0 / 1024
    c[:, C_O512:C_O512 + 128] = 1.0 / 512
    c[:, C_O128:C_O128 + 128] = 1.0 / 128
    bd = np.zeros((128, 128), np.float32)
    bd[:64, :64] = 1.0 / 64
    bd[64:, 64:] = 1.0 / 64
    c[:, C_BD64:C_BD64 + 128] = bd
    P = np.zeros((128, 128), np.float32)
    for hb in (0, 64):
        for i in range(32):
            P[hb + i, hb + i + 32] = -1.0
            P[hb + 32 + i, hb + i] = 1.0
    c[:, C_ROT:C_ROT + 128] = P.T
    c[:, C_ONE:C_ONE + 128] = 1.0
    ii = np.arange(128)
    c[:, C_SGM:C_SGM + 128] = ((ii[:, None] // 64) >= (ii[None, :] // 64)).astype(np.float32)
    inv = (10000.0 ** (-np.arange(0, 64, 2, dtype=np.float32) / 64)).astype(np.float32)
    c[:, C_INV] = inv[ii % 32]
    for gp in range(4):
        for m in range(128):
            c[2 * gp + m // 64, C_SELB + gp * 128 + m] = 1.0
    return c


_UID = [0]


def _uname(name):
    _UID[0] += 1
    return "%s_%d" % (name, _UID[0])


def T(es, nc, name, shape, dt):
    return es.enter_context(nc.sbuf_tensor(_uname(name), shape, dt))


def PS(es, nc, name):
    return es.enter_context(nc.psum_tensor(_uname(name), [128, 512], F32))


def emit_rms(S, cst, hb, hkey, gcol, xn, xkey, sq, sd, rstd, pst, tag):
    ones = cst[:, C_O1024:C_O1024 + 128]
    for c in range(8):
        b = c % 2
        S.add("act", lambda a, c=c, b=b: a.activation(out=sq[b][:], in_=hb[:, c, :], func=AF.Square),
              r=[hkey + (c,)], w=[("sg", b)])
        S.add("pe", lambda t, c=c, b=b: t.matmul(pst[:], lhsT=ones, rhs=sq[b][:], start=(c == 0), stop=(c == 7)),
              r=[("sg", b)], w=[("pst",)])
    S.add("act", lambda a: a.activation(out=sd[:], in_=pst[:], func=AF.Sqrt, bias=EPS, scale=1.0),
          r=[("pst",)], w=[("sd",)])
    S.add("dve", lambda v: v.reciprocal(out=rstd[:], in_=sd[:]), r=[("sd",)], w=[("sd",), ("rstd",)])
    for c in range(8):
        S.add("dve", lambda v, c=c: v.scalar_tensor_tensor(out=xn[:, c, :], in0=hb[:, c, :], scalar=gcol[:, c:c + 1],
                                                            in1=rstd[:], op0=ALU.mult, op1=ALU.mult),
              r=[hkey + (c,), ("rstd",)], w=[xkey + (c,)])


def phase_transpose_in(S, nc, cst, x_d, hT_d, NG):
    with ExitStack() as ph:
        xs = [T(ph, nc, "xs%d" % i, [128, 4, D], F32) for i in range(2)]
        ht = [T(ph, nc, "ht%d" % i, [128, 8, TG], F32) for i in range(2)]
        pt = [PS(ph, nc, "pt%d" % i) for i in range(4)]
        ident = cst[:, C_ID:C_ID + 128]
        xv = x_d.rearrange("(g s p) d -> g p s d", s=4, p=128)
        for g in range(NG):
            sl = g % 2
            S.dma("sp", xs[sl][:], xv[g], w=[("xs", sl)])
            for c in range(8):
                b = c % 4
                def tr(t, c=c, b=b, sl=sl):
                    for s in range(4):
                        ins = t.transpose(out=pt[b][:, s * 128:(s + 1) * 128], in_=xs[sl][:, s, c * 128:(c + 1) * 128],
                                          identity=ident)
                    return ins
                S.add("pe", tr, r=[("xs", sl)], w=[("pt", b)])
                if c % 2 == 0:
                    S.add("act", lambda a, c=c, b=b, sl=sl: a.copy(out=ht[sl][:, c, :], in_=pt[b][:]),
                          r=[("pt", b)], w=[("ht", sl, c)])
                else:
                    S.add("dve", lambda v, c=c, b=b, sl=sl: v.tensor_copy(out=ht[sl][:, c, :], in_=pt[b][:]),
                          r=[("pt", b)], w=[("ht", sl, c)])
            S.dma("pool", hT_d[:, :, g * TG:(g + 1) * TG], ht[sl][:], r=[("ht", sl, c) for c in range(8)], w=[("hTd", g)])
        S.emit()


def phase_ffn(S, nc, cst, gcol, hT_d, wg_d, wu_d, wd_d, NG):
    with ExitStack() as ph:
        Wg = T(ph, nc, "Wg", [128, 8, DFF], BF16)
        Wu = T(ph, nc, "Wu", [128, 8, DFF], BF16)
        Wd = T(ph, nc, "Wd", [128, NF, D], BF16)
        hb = [T(ph, nc, "hb%d" % i, [128, 8, TG], F32) for i in range(2)]
        xn = T(ph, nc, "xn", [128, 8, TG], BF16)
        act = T(ph, nc, "act", [128, NF, TG], BF16)
        sd = T(ph, nc, "sd", [128, TG], F32)
        rstd = sd
        sg = [T(ph, nc, "sg%d" % i, [128, TG], F32) for i in range(2)]
        sq = sg
        pst = PS(ph, nc, "pst")
        pg = [PS(ph, nc, "pg%d" % i) for i in range(2)]
        pu = [PS(ph, nc, "pu%d" % i) for i in range(2)]
        po = [PS(ph, nc, "po%d" % i) for i in range(2)]
        wgv = wg_d.rearrange("(c p) f -> p c f", p=128)
        wuv_ = wu_d.rearrange("(c p) f -> p c f", p=128)
        FB = [0, 256, 1024, 1920, DFF]
        for k in range(4):
            S.dma("pool", Wg[:, :, FB[k]:FB[k + 1]], wgv[:, :, FB[k]:FB[k + 1]], w=[("Wg", k)])
            S.dma("pool", Wu[:, :, FB[k]:FB[k + 1]], wuv_[:, :, FB[k]:FB[k + 1]], w=[("Wu", k)])
        for f in range(NF):
            S.dma("pool", Wd[:, f, :], wd_d[f * 128:(f + 1) * 128, :], w=[("Wd", f)])

        def fblk(f):
            for k in range(4):
                if f * 128 < FB[k + 1]:
                    return k
        S.dma("sp", hb[0][:], hT_d[:, :, 0:TG], w=[("h", 0, c) for c in range(8)])
        for g in range(NG):
            sl = g % 2
            if g + 1 < NG:
                S.dma("sp", hb[1 - sl][:], hT_d[:, :, (g + 1) * TG:(g + 2) * TG], w=[("h", 1 - sl, c) for c in range(8)])
            if g == 0:
                emit_rms(S, cst, hb[sl], ("h", sl), gcol, xn, ("xn",), sq, sd, rstd, pst, "f")
            xr = [("xn", c) for c in range(8)]
            for f in range(NF):
                b = f % 2
                def mmg(t, f=f, b=b):
                    for c in range(8):
                        ins = t.matmul(pg[b][:], lhsT=Wg[:, c, f * 128:(f + 1) * 128], rhs=xn[:, c, :],
                                       start=(c == 0), stop=(c == 7))
                    return ins
                def mmu(t, f=f, b=b):
                    for c in range(8):
                        ins = t.matmul(pu[b][:], lhsT=Wu[:, c, f * 128:(f + 1) * 128], rhs=xn[:, c, :],
                                       start=(c == 0), stop=(c == 7))
                    return ins
                S.add("pe", mmg, r=xr + [("Wg", fblk(f))], w=[("pg", b)])
                S.add("pe", mmu, r=xr + [("Wu", fblk(f))], w=[("pu", b)])
                S.add("act", lambda a, b=b: a.activation(out=sg[b][:], in_=pg[b][:], func=AF.Silu),
                      r=[("pg", b)], w=[("sg", b)])
                S.add("dve", lambda v, f=f, b=b: v.tensor_tensor(out=act[:, f, :], in0=pu[b][:], in1=sg[b][:], op=ALU.mult),
                      r=[("pu", b), ("sg", b)], w=[("act", f)])
            if g + 1 < NG:
                emit_rms(S, cst, hb[1 - sl], ("h", 1 - sl), gcol, xn, ("xn",), sq, sd, rstd, pst, "f")
            ar = [("act", f) for f in range(NF)]
            for m in range(8):
                b = m % 2
                def mmd(t, m=m, b=b):
                    for f in range(NF):
                        ins = t.matmul(po[b][:], lhsT=Wd[:, f, m * 128:(m + 1) * 128], rhs=act[:, f, :],
                                       start=(f == 0), stop=(f == NF - 1))
                    return ins
                S.add("pe", mmd, r=ar + [("Wd", f) for f in range(NF)], w=[("po", b)])
                S.add("dve", lambda v, m=m, b=b, sl=sl: v.scalar_tensor_tensor(
                    out=hb[sl][:, m, :], in0=po[b][:], scalar=0.5, in1=hb[sl][:, m, :], op0=ALU.mult, op1=ALU.add),
                    r=[("po", b), ("h", sl, m)], w=[("h", sl, m)])
            S.dma("pool", hT_d[:, :, g * TG:(g + 1) * TG], hb[sl][:], r=[("h", sl, c) for c in range(8)], w=[("hTd", g)])
        S.emit()


def emit_rope_tables(S, nc, cst, posb_d, g, posi, ang, tmpf, tmpi, Ct, St):
    TWO_PI = 2.0 * math.pi
    S.dma("sp", posi[:], posb_d[:, g * TG:(g + 1) * TG], w=[("posi",)])
    S.add("dve", lambda v: v.tensor_copy(out=ang[:], in_=posi[:]), r=[("posi",)], w=[("ang",)])
    S.add("dve", lambda v: v.tensor_scalar(out=ang[:], in0=ang[:], scalar1=cst[:, C_INV:C_INV + 1], scalar2=None,
                                           op0=ALU.mult), r=[("ang",)], w=[("ang",)])
    for (dst, key, shift) in ((St, "St", 0.0), (Ct, "Ct", 0.5 * math.pi)):
        S.add("dve", lambda v, shift=shift: v.tensor_scalar(out=tmpi[:], in0=ang[:], scalar1=shift, scalar2=1.0 / TWO_PI,
                                                            op0=ALU.add, op1=ALU.mult), r=[("ang",)], w=[("tmpi",)])
        S.add("dve", lambda v: v.tensor_copy(out=tmpf[:], in_=tmpi[:]), r=[("tmpi",)], w=[("tmpf",)])
        S.add("dve", lambda v: v.scalar_tensor_tensor(out=tmpf[:], in0=tmpf[:], scalar=-TWO_PI, in1=ang[:],
                                                      op0=ALU.mult, op1=ALU.add), r=[("tmpf",), ("ang",)], w=[("tmpf",)])
        S.add("dve", lambda v, shift=shift: v.tensor_scalar(out=tmpf[:], in0=tmpf[:], scalar1=shift, scalar2=None,
                                                            op0=ALU.add), r=[("tmpf",)], w=[("tmpf",)])
        S.add("dve", lambda v, dst=dst: v.tensor_scalar(out=dst[:], in0=tmpf[:], scalar1=math.pi, scalar2=-TWO_PI,
                                                        op0=ALU.is_gt, op1=ALU.mult), r=[("tmpf",)], w=[(key,)])
        S.add("dve", lambda v, dst=dst: v.tensor_tensor(out=dst[:], in0=dst[:], in1=tmpf[:], op=ALU.add),
              r=[("tmpf",), (key,)], w=[(key,)])
        S.add("dve", lambda v, dst=dst: v.tensor_scalar(out=dst[:], in0=dst[:], scalar1=-math.pi, scalar2=math.pi,
                                                        op0=ALU.max, op1=ALU.min), r=[(key,)], w=[(key,)])
        S.add("act", lambda a, dst=dst: a.activation(out=dst[:], in_=dst[:], func=AF.Sin), r=[(key,)], w=[(key,)])


def phase_evenproj(S, nc, cst, gcol, hgc, hT_d, posb_d, wfm_d, wtm_d, qkT_d, bv_d, av_d, iw_d, NG):
    with ExitStack() as ph:
        Wfm = T(ph, nc, "Wfm", [128, 8, 2048], BF16)
        Wtm = T(ph, nc, "Wtm", [128, 8, 644], BF16)
        hb = [T(ph, nc, "hb%d" % i, [128, 8, TG], F32) for i in range(2)]
        xn = T(ph, nc, "xn", [128, 8, TG], BF16)
        sd = T(ph, nc, "sd", [128, TG], F32)
        sg = [T(ph, nc, "sg%d" % i, [128, TG], F32) for i in range(2)]
        posi = T(ph, nc, "posi", [128, TG], I32)
        ang = T(ph, nc, "ang", [128, TG], F32)
        tmpf = T(ph, nc, "tmpf", [128, TG], F32)
        tmpi = T(ph, nc, "tmpi", [128, TG], I32)
        Ct = T(ph, nc, "Ct", [128, TG], F32)
        St = T(ph, nc, "St", [128, TG], F32)
        sqb = [T(ph, nc, "sqb%d" % i, [128, TG], F32) for i in range(3)]
        sdb = [T(ph, nc, "sdb%d" % i, [128, TG], F32) for i in range(3)]
        xb = [T(ph, nc, "xb%d" % i, [128, TG], F32) for i in range(3)]
        t1 = [T(ph, nc, "t1%d" % i, [128, TG], F32) for i in range(2)]
        t2 = [T(ph, nc, "t2%d" % i, [128, TG], F32) for i in range(2)]
        ob = [T(ph, nc, "ob%d" % i, [128, TG], BF16) for i in range(3)]
        bvs = [T(ph, nc, "bvs%d" % i, [128, 4, 512], BF16) for i in range(2)]
        avs = [T(ph, nc, "avs%d" % i, [128, 4, 128], BF16) for i in range(2)]
        iws = [T(ph, nc, "iws%d" % i, [128, 4, 4], F32) for i in range(2)]
        pst = PS(ph, nc, "pst")
        pta = PS(ph, nc, "pta")
        ptb = PS(ph, nc, "ptb")
        pz = [PS(ph, nc, "pz%d" % i) for i in range(3)]
        pms = PS(ph, nc, "pms")
        prot = PS(ph, nc, "prot")
        bd64 = cst[:, C_BD64:C_BD64 + 128]
        rotT = cst[:, C_ROT:C_ROT + 128]
        for c in range(8):
            S.dma("pool", Wfm[:, c, :], wfm_d[c * 128:(c + 1) * 128, :], w=[("Wfm", c)])
            S.dma("pool", Wtm[:, c, :], wtm_d[c * 128:(c + 1) * 128, :], w=[("Wtm", c)])
        wfr = [("Wfm", c) for c in range(8)]
        wtr = [("Wtm", c) for c in range(8)]
        bvv = bv_d.rearrange("(g s p) e -> g p s e", s=4, p=128)
        avv = av_d.rearrange("(g s p) e -> g p s e", s=4, p=128)
        iwv = iw_d.rearrange("(g s p) e -> g p s e", s=4, p=128)
        gidx = [0, 0, 0, 0, 1, None, None, None, 2, 2, 2, 2, 3, 3, 3, 3]
        S.dma("sp", hb[0][:], hT_d[:, :, 0:TG], w=[("h", 0, c) for c in range(8)])
        for g in range(NG):
            sl = g % 2
            if g + 1 < NG:
                S.dma("sp", hb[1 - sl][:], hT_d[:, :, (g + 1) * TG:(g + 2) * TG], w=[("h", 1 - sl, c) for c in range(8)])
            emit_rope_tables(S, nc, cst, posb_d, g, posi, ang, tmpf, tmpi, Ct, St)
            emit_rms(S, cst, hb[sl], ("h", sl), gcol, xn, ("xn",), sg, sd, sd, pst, "b")
            xr = [("xn", c) for c in range(8)]
            for s4 in range(4):
                def mma(t, s4=s4):
                    for c in range(8):
                        ins = t.matmul(pta[:], lhsT=xn[:, c, s4 * 128:(s4 + 1) * 128], rhs=Wtm[:, c, 0:512],
                                       start=(c == 0), stop=(c == 7))
                    return ins
                def mmb(t, s4=s4):
                    for c in range(8):
                        ins = t.matmul(ptb[:, 0:132], lhsT=xn[:, c, s4 * 128:(s4 + 1) * 128], rhs=Wtm[:, c, 512:644],
                                       start=(c == 0), stop=(c == 7))
                    return ins
                S.add("pe", mma, r=xr + wtr, w=[("pta",)])
                S.add("pe", mmb, r=xr + wtr, w=[("ptb",)])
                S.add("act", lambda a, s4=s4, sl=sl: a.copy(out=bvs[sl][:, s4, :], in_=pta[:]), r=[("pta",)], w=[("bvs", sl, s4)])
                S.add("dve", lambda v, s4=s4, sl=sl: v.tensor_copy(out=avs[sl][:, s4, :], in_=ptb[:, 0:128]),
                      r=[("ptb",)], w=[("avs", sl, s4)])
                S.add("dve", lambda v, s4=s4, sl=sl: v.tensor_copy(out=iws[sl][:, s4, :], in_=ptb[:, 128:132]),
                      r=[("ptb",)], w=[("iws", sl, s4)])
            S.dma("pool", bvv[g], bvs[sl][:], r=[("bvs", sl, k) for k in range(4)], w=[("bvd", g)])
            S.dma("pool", avv[g], avs[sl][:], r=[("avs", sl, k) for k in range(4)], w=[("avd", g)])
            S.dma("pool", iwv[g], iws[sl][:], r=[("iws", sl, k) for k in range(4)], w=[("iwd", g)])
            def stageA(ci):
                b = ci % 3
                def mmz(t, ci=ci, b=b):
                    for c in range(8):
                        ins = t.matmul(pz[b][:], lhsT=Wfm[:, c, ci * 128:(ci + 1) * 128], rhs=xn[:, c, :],
                                       start=(c == 0), stop=(c == 7))
                    return ins
                S.add("pe", mmz, r=xr + wfr, w=[("pz", b)])
                if gidx[ci] is not None:
                    S.add("act", lambda a, b=b: a.activation(out=sqb[b][:], in_=pz[b][:], func=AF.Square),
                          r=[("pz", b)], w=[("sqb", b)])
                else:
                    S.add("act", lambda a, b=b: a.copy(out=xb[b][:], in_=pz[b][:]), r=[("pz", b)], w=[("xb", b)])

            def stageB(ci):
                b = ci % 3
                if gidx[ci] is None:
                    return
                gi = gidx[ci]
                S.add("pe", lambda t, b=b: t.matmul(pms[:], lhsT=bd64, rhs=sqb[b][:], start=True, stop=True),
                      r=[("sqb", b)], w=[("pms",)])
                S.add("act", lambda a, b=b: a.activation(out=sdb[b][:], in_=pms[:], func=AF.Sqrt, bias=EPS, scale=1.0),
                      r=[("pms",)], w=[("sdb", b)])
                S.add("dve", lambda v, b=b: v.reciprocal(out=sdb[b][:], in_=sdb[b][:]), r=[("sdb", b)], w=[("sdb", b)])
                S.add("dve", lambda v, b=b, gi=gi: v.scalar_tensor_tensor(
                    out=xb[b][:], in0=pz[b][:], scalar=hgc[:, gi:gi + 1], in1=sdb[b][:], op0=ALU.mult, op1=ALU.mult),
                    r=[("pz", b), ("sdb", b)], w=[("xb", b)])

            def stageC(ci, g=g):
                b = ci % 3
                b2 = ci % 2
                S.add("pe", lambda t, b=b: t.matmul(prot[:], lhsT=rotT, rhs=xb[b][:], start=True, stop=True),
                      r=[("xb", b)], w=[("prot",)])
                S.add("pool", lambda v, b=b, b2=b2: v.tensor_tensor(out=t1[b2][:], in0=xb[b][:], in1=Ct[:], op=ALU.mult),
                      r=[("xb", b), ("Ct",)], w=[("t1", b2)])
                S.add("dve", lambda v, b2=b2: v.tensor_tensor(out=t2[b2][:], in0=prot[:], in1=St[:], op=ALU.mult),
                      r=[("prot",), ("St",)], w=[("t2", b2)])
                S.add("pool", lambda v, b=b, b2=b2: v.tensor_tensor(out=ob[b][:], in0=t1[b2][:], in1=t2[b2][:], op=ALU.add),
                      r=[("t1", b2), ("t2", b2)], w=[("ob", b)])
                S.dma("pool", qkT_d[ci, :, g * TG:(g + 1) * TG], ob[b][:], r=[("ob", b)], w=[("qkd", ci, g)])

            for step in range(18):
                if step < 16:
                    stageA(step)
                if 0 <= step - 1 < 16:
                    stageB(step - 1)
                if 0 <= step - 2 < 16:
                    stageC(step - 2)
        S.emit()


def phase_proj_res(S, nc, hT_d, catT_d, w_d, NG):
    with ExitStack() as ph:
        W = T(ph, nc, "Wo", [128, 8, D], BF16)
        hb = [T(ph, nc, "hb%d" % i, [128, 8, TG], F32) for i in range(2)]
        cat = [T(ph, nc, "cat%d" % i, [128, 8, TG], BF16) for i in range(2)]
        po = [PS(ph, nc, "po%d" % i) for i in range(2)]
        for c in range(8):
            S.dma("pool", W[:, c, :], w_d[c * 128:(c + 1) * 128, :], w=[("W", c)])
        cv = catT_d.rearrange("c p t -> p c t")
        for g in range(NG):
            sl = g % 2
            S.dma("sp", hb[sl][:], hT_d[:, :, g * TG:(g + 1) * TG], w=[("h", sl, c) for c in range(8)])
            S.dma("sp", cat[sl][:], cv[:, :, g * TG:(g + 1) * TG], w=[("cat", sl)])
            for m in range(8):
                b = m % 2
                def mm(t, m=m, b=b, sl=sl):
                    for c in range(8):
                        ins = t.matmul(po[b][:], lhsT=W[:, c, m * 128:(m + 1) * 128], rhs=cat[sl][:, c, :],
                                       start=(c == 0), stop=(c == 7))
                    return ins
                S.add("pe", mm, r=[("cat", sl)] + [("W", c) for c in range(8)], w=[("po", b)])
                S.add("dve", lambda v, m=m, b=b, sl=sl: v.tensor_tensor(out=hb[sl][:, m, :], in0=po[b][:], in1=hb[sl][:, m, :],
                                                                        op=ALU.add),
                      r=[("po", b), ("h", sl, m)], w=[("h", sl, m)])
            S.dma("pool", hT_d[:, :, g * TG:(g + 1) * TG], hb[sl][:], r=[("h", sl, c) for c in range(8)], w=[("hTd", g)])
        S.emit()


def phase_ple(S, nc, cst, gcol, hT_d, p_d, wgate_d, wproj_d, out_d, NG):
    with ExitStack() as ph:
        Wg = T(ph, nc, "Wgt", [128, 8, D], BF16)
        Wp = T(ph, nc, "Wpj", [128, 2, D], BF16)
        hb = [T(ph, nc, "hb%d" % i, [128, 8, TG], F32) for i in range(2)]
        xn = T(ph, nc, "xn", [128, 8, TG], BF16)
        sd = T(ph, nc, "sd", [128, TG], F32)
        sg = [T(ph, nc, "sg%d" % i, [128, TG], F32) for i in range(2)]
        ps_ = [T(ph, nc, "ps%d" % i, [128, 4, 256], F32) for i in range(2)]
        pT = T(ph, nc, "pT", [128, 2, TG], BF16)
        tt = [T(ph, nc, "tt%d" % i, [128, TG], F32) for i in range(2)]
        if out_d is not None:
            os_ = [T(ph, nc, "os%d" % i, [128, 4, D], F32) for i in range(2)]
        pst = PS(ph, nc, "pst")
        pg = [PS(ph, nc, "pg%d" % i) for i in range(2)]
        pp = [PS(ph, nc, "pp%d" % i) for i in range(2)]
        ptr = [PS(ph, nc, "ptr%d" % i) for i in range(2)]
        ident = cst[:, C_ID:C_ID + 128]
        for c in range(8):
            S.dma("pool", Wg[:, c, :], wgate_d[c * 128:(c + 1) * 128, :], w=[("Wg", c)])
        for c in range(2):
            S.dma("pool", Wp[:, c, :], wproj_d[c * 128:(c + 1) * 128, :], w=[("Wp", c)])
        pv = p_d.rearrange("(g s p) e -> g p s e", s=4, p=128)
        if out_d is not None:
            ov = out_d.rearrange("(g s p) d -> g p s d", s=4, p=128)
        for g in range(NG):
            sl = g % 2
            S.dma("sp", hb[sl][:], hT_d[:, :, g * TG:(g + 1) * TG], w=[("h", sl, c) for c in range(8)])
            S.dma("sp", ps_[sl][:], pv[g], w=[("ps", sl)])
            for c2 in range(2):
                def tr(t, c2=c2, sl=sl):
                    for s4 in range(4):
                        ins = t.transpose(out=ptr[c2][:, s4 * 128:(s4 + 1) * 128], in_=ps_[sl][:, s4, c2 * 128:(c2 + 1) * 128],
                                          identity=ident)
                    return ins
                S.add("pe", tr, r=[("ps", sl)], w=[("ptr", c2)])
                S.add("act", lambda a, c2=c2: a.copy(out=pT[:, c2, :], in_=ptr[c2][:]), r=[("ptr", c2)], w=[("pT", c2)])
            emit_rms(S, cst, hb[sl], ("h", sl), gcol, xn, ("xn",), sg, sd, sd, pst, "p")
            xr = [("xn", c) for c in range(8)]
            for m in range(8):
                b = m % 2
                def mg(t, m=m, b=b):
                    for c in range(8):
                        ins = t.matmul(pg[b][:], lhsT=Wg[:, c, m * 128:(m + 1) * 128], rhs=xn[:, c, :],
                                       start=(c == 0), stop=(c == 7))
                    return ins
                def mp(t, m=m, b=b):
                    for c in range(2):
                        ins = t.matmul(pp[b][:], lhsT=Wp[:, c, m * 128:(m + 1) * 128], rhs=pT[:, c, :],
                                       start=(c == 0), stop=(c == 1))
                    return ins
                S.add("pe", mg, r=xr + [("Wg", c) for c in range(8)], w=[("pg", b)])
                S.add("pe", mp, r=[("pT", 0), ("pT", 1), ("Wp", 0), ("Wp", 1)], w=[("pp", b)])
                S.add("act", lambda a, b=b: a.activation(out=sg[b][:], in_=pg[b][:], func=AF.Sigmoid),
                      r=[("pg", b)], w=[("sg", b)])
                S.add("dve", lambda v, b=b: v.tensor_tensor(out=tt[b][:], in0=pp[b][:], in1=sg[b][:], op=ALU.mult),
                      r=[("pp", b), ("sg", b)], w=[("tt", b)])
                S.add("dve", lambda v, m=m, b=b, sl=sl: v.tensor_tensor(out=hb[sl][:, m, :], in0=tt[b][:], in1=hb[sl][:, m, :],
                                                                        op=ALU.add),
                      r=[("tt", b), ("h", sl, m)], w=[("h", sl, m)])
            if out_d is None:
                S.dma("pool", hT_d[:, :, g * TG:(g + 1) * TG], hb[sl][:], r=[("h", sl, c) for c in range(8)], w=[("hTd", g)])
            else:
                for s4 in range(4):
                    for half in range(2):
                        b = half
                        def trb(t, s4=s4, half=half, b=b, sl=sl):
                            for k in range(4):
                                c = half * 4 + k
                                ins = t.transpose(out=ptr[b][:, k * 128:(k + 1) * 128],
                                                  in_=hb[sl][:, c, s4 * 128:(s4 + 1) * 128], identity=ident)
                            return ins
                        S.add("pe", trb, r=[("h", sl, half * 4 + k) for k in range(4)], w=[("ptr", b)])
                        if half == 0:
                            S.add("act", lambda a, s4=s4, b=b, sl=sl: a.copy(out=os_[sl][:, s4, 0:512], in_=ptr[b][:]),
                                  r=[("ptr", b)], w=[("os", sl, s4, 0)])
                        else:
                            S.add("dve", lambda v, s4=s4, b=b, sl=sl: v.tensor_copy(out=os_[sl][:, s4, 512:1024], in_=ptr[b][:]),
                                  r=[("ptr", b)], w=[("os", sl, s4, 1)])
                S.dma("pool", ov[g], os_[sl][:], r=[("os", sl, a, b_) for a in range(4) for b_ in range(2)], w=[("outd", g)])
        S.emit()


def phase_diff(S, nc, cst, hgc, lamv_d, qkT_d, bv_d, catT_d, S_LEN):
    NT = S_LEN // 128
    NG = S_LEN // TG
    lam_init = 0.8 - 0.6 * math.exp(-0.3 * 0)
    U32 = mybir.dt.uint32
    with ExitStack() as ph:
        kT = T(ph, nc, "kT", [128, S_LEN], BF16)
        vT = T(ph, nc, "vT", [128, NT, 128], BF16)
        qT = [T(ph, nc, "qT%d" % i, [128, TG], BF16) for i in range(2)]
        E = [[T(ph, nc, "E%d%d" % (cm, b), [128, TG], BF16) for b in range(2)] for cm in range(2)]
        onesb = T(ph, nc, "onesb", [128, 128], BF16)
        lamv = T(ph, nc, "lamv", [128, 4, 64], F32)
        lt = T(ph, nc, "lt", [128, 2, 64], F32)
        ls = T(ph, nc, "ls", [128, 2], F32)
        neglam = T(ph, nc, "neglam", [128, 1], F32)
        gsub = T(ph, nc, "gsub", [128, 1], F32)
        rd = [T(ph, nc, "rd%d" % i, [128, TG], F32) for i in range(2)]
        t0 = T(ph, nc, "t0", [128, TG], F32)
        t1 = T(ph, nc, "t1", [128, TG], F32)
        oo = T(ph, nc, "oo", [128, TG], F32)
        sq = T(ph, nc, "sq", [128, TG], F32)
        sdd = T(ph, nc, "sdd", [128, TG], F32)
        obf = [T(ph, nc, "obf%d" % i, [128, TG], BF16) for i in range(2)]
        pl = [[PS(ph, nc, "pl%d%d" % (cm, b)) for b in range(2)] for cm in range(2)]
        pO = [PS(ph, nc, "pO%d" % i) for i in range(2)]
        pD = [PS(ph, nc, "pD%d" % i) for i in range(2)]
        o128 = cst[:, C_O128:C_O128 + 128]
        S.dma("sp", lamv[:], lamv_d, w=[("lamv",)])
        S.add("dve", lambda v: v.memset(onesb[:], 1.0), w=[("onesb",)])
        S.add("dve", lambda v: v.tensor_tensor(out=lt[:, 0, :], in0=lamv[:, 0, :], in1=lamv[:, 1, :], op=ALU.mult),
              r=[("lamv",)], w=[("lt", 0)])
        S.add("dve", lambda v: v.tensor_tensor(out=lt[:, 1, :], in0=lamv[:, 2, :], in1=lamv[:, 3, :], op=ALU.mult),
              r=[("lamv",)], w=[("lt", 1)])
        S.add("dve", lambda v: v.reduce_sum(out=ls[:, 0:1], in_=lt[:, 0, :], axis=AX.X), r=[("lt", 0)], w=[("ls", 0)])
        S.add("dve", lambda v: v.reduce_sum(out=ls[:, 1:2], in_=lt[:, 1, :], axis=AX.X), r=[("lt", 1)], w=[("ls", 1)])
        S.add("act", lambda a: a.activation(out=ls[:], in_=ls[:], func=AF.Exp), r=[("ls", 0), ("ls", 1)], w=[("ls", 0), ("ls", 1)])
        S.add("dve", lambda v: v.tensor_tensor(out=neglam[:], in0=ls[:, 1:2], in1=ls[:, 0:1], op=ALU.subtract),
              r=[("ls", 0), ("ls", 1)], w=[("neglam",)])
        S.add("dve", lambda v: v.tensor_scalar(out=neglam[:], in0=neglam[:], scalar1=-lam_init, scalar2=None, op0=ALU.add),
              r=[("neglam",)], w=[("neglam",)])
        S.add("dve", lambda v: v.tensor_scalar(out=gsub[:], in0=hgc[:, 4:5], scalar1=1.0 - lam_init, scalar2=None, op0=ALU.mult),
              r=[("hgc",)], w=[("gsub",)])
        bvv = bv_d.rearrange("(n p) e -> p n e", p=128)
        for h in range(4):
            S.dma("sp", kT[:], qkT_d[12 + h], w=[("kT",)])
            for n0 in range(0, NT, 8):
                n1 = min(NT, n0 + 8)
                S.dma("sp", vT[:, n0:n1, :], bvv[:, n0:n1, h * 128:(h + 1) * 128], w=[("vT", n0)])
            for qg in range(NG):
                qs = qg % 2
                S.dma("sp", qT[qs][:], qkT_d[8 + h, :, qg * TG:(qg + 1) * TG], w=[("qT", qs)])
                nkt = 4 * qg + 4

                def emitL(kt, qg=qg, qs=qs):
                    r_ = kt - 4 * qg
                    c0 = max(0, r_) * 128
                    b = kt % 2
                    for cm in range(2):
                        S.add("pe", lambda t, cm=cm, kt=kt, c0=c0, b=b: t.matmul(
                            pl[cm][b][:, c0:TG], lhsT=kT[cm * 64:(cm + 1) * 64, kt * 128:(kt + 1) * 128],
                            rhs=qT[qs][cm * 64:(cm + 1) * 64, c0:TG], start=True, stop=True),
                            r=[("kT",), ("qT", qs)], w=[("pl", cm, b)])
                        S.add("act", lambda a, cm=cm, c0=c0, b=b: a.activation(
                            out=E[cm][b][:, c0:TG], in_=pl[cm][b][:, c0:TG], func=AF.Exp, scale=0.125),
                            r=[("pl", cm, b)], w=[("E", cm, b)])
                        if r_ >= 0:
                            S.add("pool", lambda p_, cm=cm, c0=c0, b=b: p_.memset(E[cm][b][64:128, c0:c0 + 64], 0.0),
                                  r=[("E", cm, b)], w=[("E", cm, b)])

                def emitPV(kt, qg=qg, nkt=nkt):
                    c0 = max(0, kt - 4 * qg) * 128
                    b = kt % 2
                    for cm in range(2):
                        def pv(t, cm=cm, kt=kt, c0=c0, b=b):
                            t.matmul(pO[cm][:, c0:TG], lhsT=vT[:, kt, :], rhs=E[cm][b][:, c0:TG],
                                     start=(kt == 0), stop=(kt == nkt - 1))
                            return t.matmul(pD[cm][:, c0:TG], lhsT=onesb[:], rhs=E[cm][b][:, c0:TG],
                                            start=(kt == 0), stop=(kt == nkt - 1))
                        S.add("pe", pv, r=[("E", cm, b), ("vT", (kt // 8) * 8), ("onesb",)], w=[("pO", cm), ("pD", cm)])

                emitL(0)
                for kt in range(nkt):
                    if kt + 1 < nkt:
                        emitL(kt + 1)
                    emitPV(kt)
                for cm in range(2):
                    S.add("dve", lambda v, cm=cm: v.reciprocal(out=rd[cm][:], in_=pD[cm][:]), r=[("pD", cm)], w=[("rd", cm)])
                S.add("dve", lambda v: v.tensor_tensor(out=t0[:], in0=pO[0][:], in1=rd[0][:], op=ALU.mult),
                      r=[("pO", 0), ("rd", 0)], w=[("t0",)])
                S.add("dve", lambda v: v.tensor_scalar(out=rd[1][:], in0=rd[1][:], scalar1=neglam[:, 0:1], scalar2=None, op0=ALU.mult),
                      r=[("rd", 1), ("neglam",)], w=[("rd", 1)])
                S.add("dve", lambda v: v.tensor_tensor(out=t1[:], in0=pO[1][:], in1=rd[1][:], op=ALU.mult),
                      r=[("pO", 1), ("rd", 1)], w=[("t1",)])
                S.add("dve", lambda v: v.tensor_tensor(out=oo[:], in0=t0[:], in1=t1[:], op=ALU.add),
                      r=[("t0",), ("t1",)], w=[("oo",)])
                S.add("act", lambda a: a.activation(out=sq[:], in_=oo[:], func=AF.Square), r=[("oo",)], w=[("sq",)])
                S.add("pe", lambda t: t.matmul(pl[0][0][:], lhsT=o128, rhs=sq[:], start=True, stop=True),
                      r=[("sq",)], w=[("pl", 0, 0)])
                S.add("act", lambda a: a.activation(out=sdd[:], in_=pl[0][0][:], func=AF.Sqrt, bias=EPS, scale=1.0),
                      r=[("pl", 0, 0)], w=[("sdd",)])
                S.add("dve", lambda v: v.reciprocal(out=sdd[:], in_=sdd[:]), r=[("sdd",)], w=[("sdd",)])
                S.add("dve", lambda v, qs=qs: v.scalar_tensor_tensor(out=obf[qs][:], in0=oo[:], scalar=gsub[:, 0:1], in1=sdd[:],
                                                                     op0=ALU.mult, op1=ALU.mult),
                      r=[("oo",), ("sdd",), ("gsub",)], w=[("obf", qs)])
                S.dma("pool", catT_d[4 + h, :, qg * TG:(qg + 1) * TG], obf[qs][:], r=[("obf", qs)], w=[("catd", h, qg)])
        S.emit()


def phase_dsa(S, nc, cst, qkT_d, av_d, iw_d, wuv_d, catT_d, S_LEN):
    NT = S_LEN // 128
    NITER = 14
    TOPK = 256.0
    BIGM = 30000.0
    with ExitStack() as ph:
        akT = T(ph, nc, "akT", [128, S_LEN], BF16)
        ikT = T(ph, nc, "ikT", [128, S_LEN], BF16)
        vT = T(ph, nc, "avT", [128, NT, 128], BF16)
        wuv = T(ph, nc, "wuv", [128, 8, 128], BF16)
        irep = T(ph, nc, "irep", [128, 512], BF16)
        onesb = T(ph, nc, "onesb2", [128, 128], BF16)
        sc = T(ph, nc, "sc", [128, S_LEN], F32)
        mb = [T(ph, nc, "mb%d" % i, [128, S_LEN], BF16) for i in range(2)]
        cand = T(ph, nc, "cand", [128, S_LEN], BF16)
        rank = T(ph, nc, "rank", [128, S_LEN], F32)
        hip = T(ph, nc, "hip", [128, 1], F32)
        wt = T(ph, nc, "wt", [128, NITER], F32)
        nhw = T(ph, nc, "nhw", [128, NITER], F32)
        ftab = T(ph, nc, "ftab", [128, NITER], F32)
        chi = T(ph, nc, "chi", [128, 1], F32)
        rr = T(ph, nc, "rr", [128, 1], F32)
        aq = [T(ph, nc, "aq%d" % i, [128, 4, 128], BF16) for i in range(2)]
        iq = [T(ph, nc, "iq%d" % i, [128, 2, 128], BF16) for i in range(2)]
        iw = [T(ph, nc, "iw%d" % i, [128, 4], F32) for i in range(2)]
        rl = [T(ph, nc, "rl%d" % i, [128, 512], F32) for i in range(2)]
        lo = T(ph, nc, "lo", [128, 1], F32)
        hi = T(ph, nc, "hi", [128, 1], F32)
        w0 = T(ph, nc, "w0", [128, 1], F32)
        mid = T(ph, nc, "mid", [128, 1], F32)
        cnt = T(ph, nc, "cnt", [128, 1], F32)
        g2 = T(ph, nc, "g2", [128, 1], F32)
        tmn = T(ph, nc, "tmn", [128, 1], F32)
        E = [[T(ph, nc, "Ea%d%d" % (hg, b), [128, 512], BF16) for b in range(2)] for hg in range(2)]
        rD = T(ph, nc, "rD", [128, 512], F32)
        olat = T(ph, nc, "olat", [128, 2, 512], BF16)
        ao = [T(ph, nc, "ao%d" % i, [128, 4, 128], BF16) for i in range(2)]
        pd = [PS(ph, nc, "pd%d" % i) for i in range(2)]
        pl = [PS(ph, nc, "pla%d" % i) for i in range(2)]
        pO = [PS(ph, nc, "pOa%d" % i) for i in range(2)]
        pD = [PS(ph, nc, "pDa%d" % i) for i in range(2)]
        ident = cst[:, C_ID:C_ID + 128]
        S.add("pool", lambda p_: p_.memset(wuv[:], 0.0), w=[("wuvz",)])
        for h in range(8):
            S.dma("pool", wuv[:, h, (h % 2) * 64:(h % 2) * 64 + 64], wuv_d[h], r=[("wuvz",)], w=[("wuv", h)])
        wuvr = [("wuv", h) for h in range(8)]
        for k in range(4):
            S.add("act", lambda a, k=k: a.mul(out=irep[:, k * 128:(k + 1) * 128], in_=ident, mul=BIGM), r=[("cst",)], w=[("irep", k)])
        irr = [("irep", k) for k in range(4)]
        S.add("dve", lambda v: v.memset(onesb[:], 1.0), w=[("onesb",)])
        for it in range(NITER):
            S.add("pool", lambda p_, it=it: p_.memset(ftab[:, it:it + 1], 0.5 ** (it + 1)), w=[("ftab",)])
        S.dma("sp", akT[:], qkT_d[4], w=[("akT",)])
        S.dma("sp", ikT[:], qkT_d[7], w=[("ikT",)])
        avv_ = av_d.rearrange("(n p) e -> p n e", p=128)
        for n0 in range(0, NT, 8):
            n1 = min(NT, n0 + 8)
            S.dma("sp", vT[:, n0:n1, :], avv_[:, n0:n1, :], w=[("vT", n0)])
        def front(j):
            s = j % 2
            NK = (j + 1) * 128
            js = slice(j * 128, (j + 1) * 128)
            S.dma("sp", aq[s][:], qkT_d[0:4, :, js].rearrange("c p q -> p c q"), w=[("aq", s)])
            S.dma("sp", iq[s][:], qkT_d[5:7, :, js].rearrange("c p q -> p c q"), w=[("iq", s)])
            S.dma("sp", iw[s][:], iw_d[js, :], w=[("iw", s)])
            ntile = (NK + 511) // 512
            sck = [("sc", t) for t in range(ntile)]
            for t in range(ntile):
                w_ = min(512, NK - 512 * t)
                for h in range(4):
                    b = h % 2
                    rs = slice((h % 2) * 64, (h % 2) * 64 + 64)
                    S.add("pe", lambda t_, h=h, b=b, rs=rs, t=t, w_=w_, s=s: t_.matmul(
                        pd[b][:, 0:w_], lhsT=iq[s][rs, h // 2, :], rhs=ikT[rs, t * 512:t * 512 + w_], start=True, stop=True),
                        r=[("iq", s), ("ikT",)], w=[("pd", b)])
                    S.add("act", lambda a, b=b, w_=w_: a.activation(out=rl[b][:, 0:w_], in_=pd[b][:, 0:w_], func=AF.Relu),
                          r=[("pd", b)], w=[("rl", b)])
                    if h == 0:
                        S.add("dve", lambda v, b=b, t=t, w_=w_, s=s: v.tensor_scalar(
                            out=sc[:, t * 512:t * 512 + w_], in0=rl[b][:, 0:w_], scalar1=iw[s][:, 0:1], scalar2=None, op0=ALU.mult),
                            r=[("rl", b), ("iw", s)], w=[("sc", t)])
                    else:
                        S.add("dve", lambda v, b=b, t=t, w_=w_, s=s, h=h: v.scalar_tensor_tensor(
                            out=sc[:, t * 512:t * 512 + w_], in0=rl[b][:, 0:w_], scalar=iw[s][:, h:h + 1],
                            in1=sc[:, t * 512:t * 512 + w_], op0=ALU.mult, op1=ALU.add),
                            r=[("rl", b), ("iw", s), ("sc", t)], w=[("sc", t)])
            S.add("dve", lambda v, NK=NK: v.memset(sc[0:64, NK - 64:NK], -1.0e30), r=[("sc", ntile - 1)], w=[("sc", ntile - 1)])
            S.add("dve", lambda v, NK=NK: v.reduce_max(out=hi[:], in_=sc[:, 0:NK], axis=AX.X), r=sck, w=[("hi",)])
            S.add("dve", lambda v, NK=NK: v.tensor_reduce(out=lo[:], in_=sc[:, 0:NK - 64], axis=AX.X, op=ALU.min), r=sck, w=[("lo",)])
            S.add("dve", lambda v, NK=NK: v.tensor_reduce(out=tmn[64:128, :], in_=sc[64:128, NK - 64:NK], axis=AX.X, op=ALU.min),
                  r=sck, w=[("tmn",)])
            S.add("dve", lambda v: v.tensor_tensor(out=lo[64:128, :], in0=lo[64:128, :], in1=tmn[64:128, :], op=ALU.min),
                  r=[("lo",), ("tmn",)], w=[("lo",)])
            S.add("dve", lambda v: v.tensor_scalar(out=lo[:], in0=lo[:], scalar1=-1.0, scalar2=None, op0=ALU.add),
                  r=[("lo",)], w=[("lo",)])
            S.add("dve", lambda v: v.tensor_tensor(out=w0[:], in0=hi[:], in1=lo[:], op=ALU.subtract),
                  r=[("lo",), ("hi",)], w=[("w0",)])
            S.add("dve", lambda v: v.tensor_scalar(out=wt[:], in0=ftab[:], scalar1=w0[:, 0:1], scalar2=None, op0=ALU.mult),
                  r=[("w0",), ("ftab",)], w=[("wt",)])
            S.add("dve", lambda v: v.tensor_scalar(out=nhw[:], in0=wt[:], scalar1=-0.5, scalar2=None, op0=ALU.mult),
                  r=[("wt",)], w=[("nhw",)])
            S.add("dve", lambda v: v.tensor_tensor(out=mid[:], in0=lo[:], in1=wt[:, 0:1], op=ALU.add),
                  r=[("lo",), ("wt",)], w=[("mid",)])
            for it in range(NITER):
                S.add("dve", lambda v, NK=NK: v.tensor_scalar(out=mb[s][:, 0:NK], in0=sc[:, 0:NK], scalar1=mid[:, 0:1], scalar2=None,
                                                              op0=ALU.is_gt, op1=ALU.add, accum_out=cnt[:]),
                      r=sck + [("mid",)], w=[("mb", s), ("cnt",)])
                S.add("dve", lambda v, it=it: v.tensor_scalar(out=g2[:], in0=cnt[:], scalar1=TOPK, scalar2=wt[:, it:it + 1],
                                                              op0=ALU.is_ge, op1=ALU.mult),
                      r=[("cnt",), ("wt",)], w=[("g2",)])
                S.add("dve", lambda v, it=it: v.scalar_tensor_tensor(out=mid[:], in0=g2[:], scalar=nhw[:, it:it + 1], in1=mid[:],
                                                                     op0=ALU.add, op1=ALU.add),
                      r=[("g2",), ("nhw",), ("mid",)], w=[("mid",)])
            fe = 0.5 ** (NITER + 1)
            S.add("dve", lambda v: v.scalar_tensor_tensor(out=lo[:], in0=w0[:], scalar=-fe, in1=mid[:], op0=ALU.mult, op1=ALU.add),
                  r=[("w0",), ("mid",)], w=[("lo",)])
            S.add("dve", lambda v: v.scalar_tensor_tensor(out=hip[:], in0=w0[:], scalar=fe, in1=mid[:], op0=ALU.mult, op1=ALU.add),
                  r=[("w0",), ("mid",)], w=[("hip",)])
            S.add("dve", lambda v, NK=NK: v.tensor_scalar(out=mb[s][:, 0:NK], in0=sc[:, 0:NK], scalar1=hip[:, 0:1], scalar2=None,
                                                          op0=ALU.is_gt, op1=ALU.add, accum_out=chi[:]),
                  r=sck + [("hip",)], w=[("mb", s), ("chi",)])
            S.add("dve", lambda v: v.tensor_scalar(out=rr[:], in0=chi[:], scalar1=-1.0, scalar2=TOPK, op0=ALU.mult, op1=ALU.add),
                  r=[("chi",)], w=[("rr",)])
            S.add("dve", lambda v, NK=NK: v.scalar_tensor_tensor(out=cand[:, 0:NK], in0=sc[:, 0:NK], scalar=lo[:, 0:1], in1=mb[s][:, 0:NK],
                                                                 op0=ALU.is_gt, op1=ALU.subtract),
                  r=sck + [("lo",), ("mb", s)], w=[("cand",)])
            S.add("dve", lambda v, NK=NK: v.tensor_tensor_scan(out=rank[:, 0:NK], data0=cand[:, 0:NK], data1=cand[:, 0:NK], initial=0.0,
                                                               op0=ALU.add, op1=ALU.max),
                  r=[("cand",)], w=[("rank",)])
            S.add("dve", lambda v, NK=NK: v.scalar_tensor_tensor(out=cand[:, 0:NK], in0=rank[:, 0:NK], scalar=rr[:, 0:1], in1=cand[:, 0:NK],
                                                                 op0=ALU.is_le, op1=ALU.mult),
                  r=[("rank",), ("rr",), ("cand",)], w=[("cand",)])
            S.add("dve", lambda v, NK=NK: v.scalar_tensor_tensor(out=mb[s][:, 0:NK], in0=mb[s][:, 0:NK], scalar=-1.0, in1=cand[:, 0:NK],
                                                                 op0=ALU.add, op1=ALU.add),
                  r=[("mb", s), ("cand",)], w=[("mb", s)])
        def back(j):
            s = j % 2
            js = slice(j * 128, (j + 1) * 128)
            for kt in range(j + 1):
                b = kt % 2
                for hg in range(2):
                    rs = slice(hg * 64, hg * 64 + 64)
                    def lg(t_, hg=hg, rs=rs, kt=kt, s=s):
                        t_.matmul(pl[hg][:], lhsT=akT[rs, kt * 128:(kt + 1) * 128],
                                  rhs=aq[s][rs, :, :].rearrange("p c q -> p (c q)"), start=True, stop=False)
                        return t_.matmul(pl[hg][:], lhsT=mb[s][:, kt * 128:(kt + 1) * 128], rhs=irep[:], start=False, stop=True)
                    S.add("pe", lg, r=[("akT",), ("aq", s), ("mb", s)] + irr, w=[("pl", hg)])
                    S.add("act", lambda a, hg=hg, b=b: a.activation(out=E[hg][b][:], in_=pl[hg][:], func=AF.Exp, scale=0.125),
                          r=[("pl", hg)], w=[("E", hg, b)])
                for hg in range(2):
                    def pv(t_, hg=hg, kt=kt, b=b, j=j):
                        t_.matmul(pO[hg][:], lhsT=vT[:, kt, :], rhs=E[hg][b][:], start=(kt == 0), stop=(kt == j))
                        return t_.matmul(pD[hg][:], lhsT=onesb[:], rhs=E[hg][b][:], start=(kt == 0), stop=(kt == j))
                    S.add("pe", pv, r=[("E", hg, b), ("vT", (kt // 8) * 8), ("onesb",)], w=[("pO", hg), ("pD", hg)])
            for hg in range(2):
                S.add("dve", lambda v, hg=hg: v.reciprocal(out=rD[:], in_=pD[hg][:]), r=[("pD", hg)], w=[("rD",)])
                S.add("dve", lambda v, hg=hg: v.tensor_tensor(out=olat[:, hg, :], in0=pO[hg][:], in1=rD[:], op=ALU.mult),
                      r=[("pO", hg), ("rD",)], w=[("olat", hg)])
            def uv(t_):
                for i in range(4):
                    t_.matmul(pd[0][:, i * 128:(i + 1) * 128], lhsT=wuv[:, 2 * i, :], rhs=olat[:, 0, i * 128:(i + 1) * 128],
                              start=True, stop=False)
                    ins = t_.matmul(pd[0][:, i * 128:(i + 1) * 128], lhsT=wuv[:, 2 * i + 1, :], rhs=olat[:, 1, i * 128:(i + 1) * 128],
                                    start=False, stop=True)
                return ins
            S.add("pe", uv, r=[("olat", 0), ("olat", 1)] + wuvr, w=[("pd", 0)])
            S.add("act", lambda a, s=s: a.copy(out=ao[s][:].rearrange("p c q -> p (c q)"), in_=pd[0][:]), r=[("pd", 0)], w=[("ao", s)])
            S.dma("pool", catT_d[0:4, :, js].rearrange("c p q -> p c q"), ao[s][:], r=[("ao", s)], w=[("catd", j)])
        front(0)
        for j in range(NT):
            if j + 1 < NT:
                front(j + 1)
            back(j)
        S.emit()


def phase_odd(S, nc, cst, gcol, hT_d, win_d, wout_d, oddc_d, oddb_d, cws_d, cbs_d, NG):
    with ExitStack() as ph:
        Win = T(ph, nc, "Win", [128, 8, 2048], BF16)
        Wout = T(ph, nc, "Wout", [128, 8, D], BF16)
        diag = T(ph, nc, "diag", [128, 4, 31, 128], BF16)
        WsT = T(ph, nc, "WsT", [128, 8, 128], BF16)
        cbs = T(ph, nc, "cbs", [8, 128], F32)
        oddc = T(ph, nc, "oddc", [128, 16 + 4 * 31], F32)
        oddb = T(ph, nc, "oddb", [128, 2, 4, 2, 64], F32)
        wsl = [T(ph, nc, "wsl%d" % i, [128, 128], F32) for i in range(2)]
        hb = [T(ph, nc, "hb%d" % i, [128, 8, TG], F32) for i in range(2)]
        xn = T(ph, nc, "xn", [128, 8, TG], BF16)
        sd = T(ph, nc, "sd", [128, TG], F32)
        sg = [T(ph, nc, "sg%d" % i, [128, TG], F32) for i in range(2)]
        uT = T(ph, nc, "uT", [128, 4, TG], BF16)
        cat = T(ph, nc, "cat", [128, 8, TG], BF16)
        vt = [T(ph, nc, "vt%d" % i, [128, 4, 2, 64], F32) for i in range(2)]
        vnp = [T(ph, nc, "vnp%d" % i, [128, 4, 2, 128], BF16) for i in range(2)]
        st = T(ph, nc, "st", [128, 6], F32)
        mv = T(ph, nc, "mv", [128, 2], F32)
        sdv = T(ph, nc, "sdv", [128, 1], F32)
        hdb = T(ph, nc, "hdb", [128, 4, 30 + TG], BF16)
        y = T(ph, nc, "y", [128, 4, TG], F32)
        mu = T(ph, nc, "mu", [128, TG], F32)
        rs2 = T(ph, nc, "rs2", [128, TG], F32)
        tmp = [T(ph, nc, "tmp%d" % i, [128, TG], F32) for i in range(2)]
        pst = PS(ph, nc, "pst")
        pz = [PS(ph, nc, "pz%d" % i) for i in range(2)]
        pg = [PS(ph, nc, "pg%d" % i) for i in range(2)]
        pvt = PS(ph, nc, "pvt")
        psg = PS(ph, nc, "psg")
        pc = PS(ph, nc, "pc")
        ident = cst[:, C_ID:C_ID + 128]
        o512 = cst[:, C_O512:C_O512 + 128]
        for c in range(8):
            S.dma("pool", Win[:, c, :], win_d[c * 128:(c + 1) * 128, :], w=[("Win", c)])
        for c in range(8):
            S.dma("pool", Wout[:, c, :], wout_d[c * 128:(c + 1) * 128, :], w=[("Wout", c)])
        wir = [("Win", c) for c in range(8)]
        wor = [("Wout", c) for c in range(8)]
        S.dma("sp", oddc[:], oddc_d, w=[("oddc",)])
        S.dma("sp", oddb[:].rearrange("p a g t c -> p a (g t c)"), oddb_d, w=[("oddb",)])
        S.dma("sp", cbs[:], cbs_d, w=[("cbs",)])
        for ch in range(4):
            for k in range(31):
                eng = "dve" if (ch * 31 + k) % 2 == 0 else "pool"
                S.add(eng, lambda e, ch=ch, k=k: e.tensor_scalar(out=diag[:, ch, k, :], in0=ident,
                                                                 scalar1=oddc[:, 16 + ch * 31 + k:16 + ch * 31 + k + 1],
                                                                 scalar2=None, op0=ALU.mult),
                      r=[("oddc",), ("cst",)], w=[("diag", ch, k)])
        for g_ in range(8):
            b = g_ % 2
            S.dma("sp", wsl[b][:], cws_d[g_], w=[("wsl", b)])
            S.add("dve", lambda v, b=b: v.tensor_tensor(out=wsl[b][:], in0=wsl[b][:], in1=cst[:, C_SGM:C_SGM + 128], op=ALU.mult),
                  r=[("wsl", b)], w=[("wsl", b)])
            S.add("pe", lambda t, b=b: t.transpose(out=pvt[:, 0:128], in_=wsl[b][:], identity=ident), r=[("wsl", b)], w=[("pvt",)])
            S.add("act", lambda a, g_=g_: a.copy(out=WsT[:, g_, :], in_=pvt[:, 0:128]), r=[("pvt",)], w=[("WsT", g_)])
        wsr = [("WsT", g_) for g_ in range(8)]
        for i in range(2):
            S.add("pool", lambda p_, i=i: p_.memset(vnp[i][:], 0.0), w=[("vnp", i)])
        S.add("pool", lambda p_: p_.memset(hdb[:], 0.0), w=[("hdb", ch) for ch in range(4)])
        S.dma("sp", hb[0][:], hT_d[:, :, 0:TG], w=[("h", 0, c) for c in range(8)])
        for g in range(NG):
            sl = g % 2
            if g + 1 < NG:
                S.dma("sp", hb[1 - sl][:], hT_d[:, :, (g + 1) * TG:(g + 2) * TG], w=[("h", 1 - sl, c) for c in range(8)])
            emit_rms(S, cst, hb[sl], ("h", sl), gcol, xn, ("xn",), sg, sd, sd, pst, "o")
            xr = [("xn", c) for c in range(8)]
            for ci in range(4):
                b = ci % 2
                def mmu(t, ci=ci, b=b):
                    for c in range(8):
                        ins = t.matmul(pz[b][:], lhsT=Win[:, c, ci * 128:(ci + 1) * 128], rhs=xn[:, c, :], start=(c == 0), stop=(c == 7))
                    return ins
                S.add("pe", mmu, r=xr + wir, w=[("pz", b)])
                S.add("act", lambda a, ci=ci, b=b: a.activation(out=uT[:, ci, :], in_=pz[b][:], func=AF.Gelu), r=[("pz", b)], w=[("uT", ci)])
            def v_pre(s4):
                b = s4 % 2
                ts_ = slice(s4 * 128, (s4 + 1) * 128)
                def mmv(t, ts_=ts_):
                    for c in range(8):
                        ins = t.matmul(pvt[:], lhsT=xn[:, c, ts_], rhs=Win[:, c, 512:1024], start=(c == 0), stop=(c == 7))
                    return ins
                S.add("pe", mmv, r=xr + wir, w=[("pvt",)])
                vflat = vt[b][:].rearrange("p g t c -> p (g t c)")
                S.add("act", lambda a, vflat=vflat: a.activation(out=vflat, in_=pvt[:], func=AF.Gelu), r=[("pvt",)], w=[("vt", b)])
                S.add("dve", lambda v, vflat=vflat: v.bn_stats(out=st[:], in_=vflat), r=[("vt", b)], w=[("st",)])
                S.add("dve", lambda v: v.bn_aggr(out=mv[:], in_=st[:]), r=[("st",)], w=[("mv",)])
                S.add("act", lambda a: a.activation(out=sdv[:], in_=mv[:, 1:2], func=AF.Sqrt, bias=EPS, scale=1.0), r=[("mv",)], w=[("sdv",)])
                S.add("dve", lambda v: v.reciprocal(out=sdv[:], in_=sdv[:]), r=[("sdv",)], w=[("sdv",)])
                S.add("dve", lambda v, vflat=vflat: v.tensor_scalar(out=vflat, in0=vflat, scalar1=mv[:, 0:1], scalar2=sdv[:, 0:1],
                                                                    op0=ALU.subtract, op1=ALU.mult),
                      r=[("vt", b), ("mv",), ("sdv",)], w=[("vt", b)])
                S.add("dve", lambda v, b=b: v.tensor_tensor(out=vt[b][:], in0=vt[b][:], in1=oddb[:, 0], op=ALU.mult),
                      r=[("vt", b), ("oddb",)], w=[("vt", b)])
                S.add("dve", lambda v, b=b: v.tensor_tensor(out=vnp[b][:, :, 0, 0:64], in0=vt[b][:, :, 0, :], in1=oddb[:, 1, :, 0, :], op=ALU.add),
                      r=[("vt", b), ("oddb",)], w=[("vnp", b)])
                S.add("dve", lambda v, b=b: v.tensor_tensor(out=vnp[b][:, :, 1, 64:128], in0=vt[b][:, :, 1, :], in1=oddb[:, 1, :, 1, :], op=ALU.add),
                      r=[("vt", b), ("oddb",)], w=[("vnp", b)])
            def v_post(s4):
                b = s4 % 2
                ts_ = slice(s4 * 128, (s4 + 1) * 128)
                def sgu(t, b=b):
                    for gp in range(4):
                        o_ = psg[:, gp * 128:(gp + 1) * 128]
                        t.matmul(o_, lhsT=vnp[b][:, gp, 0, :], rhs=WsT[:, 2 * gp, :], start=True, stop=False)
                        t.matmul(o_, lhsT=vnp[b][:, gp, 1, :], rhs=WsT[:, 2 * gp + 1, :], start=False, stop=False)
                        ins = t.matmul(o_, lhsT=cst[0:8, C_SELB + gp * 128:C_SELB + (gp + 1) * 128], rhs=cbs[:, :], start=False, stop=True)
                    return ins
                S.add("pe", sgu, r=[("vnp", b), ("cbs",), ("cst",)] + wsr, w=[("psg",)])
                S.add("dve", lambda v, ts_=ts_: v.tensor_tensor(out=cat[:, 0:4, ts_], in0=psg[:].rearrange("p (g i) -> p g i", g=4),
                                                                in1=uT[:, :, ts_], op=ALU.mult),
                      r=[("psg",)] + [("uT", ci) for ci in range(4)], w=[("cat", s4)])
            def conv_part(ch):
                b = ch % 2
                def mma(t, ch=ch, b=b):
                    for c in range(8):
                        ins = t.matmul(pz[b][:], lhsT=Win[:, c, 1024 + ch * 128:1024 + (ch + 1) * 128], rhs=xn[:, c, :],
                                       start=(c == 0), stop=(c == 7))
                    return ins
                def mmg(t, ch=ch, b=b):
                    for c in range(8):
                        ins = t.matmul(pg[b][:], lhsT=Win[:, c, 1536 + ch * 128:1536 + (ch + 1) * 128], rhs=xn[:, c, :],
                                       start=(c == 0), stop=(c == 7))
                    return ins
                S.add("pe", mma, r=xr + wir, w=[("pz", b)])
                S.add("pe", mmg, r=xr + wir, w=[("pg", b)])
                S.add("act", lambda a, b=b: a.activation(out=sg[b][:], in_=pg[b][:], func=AF.Sigmoid), r=[("pg", b)], w=[("sg", b)])
                S.add("dve", lambda v, ch=ch, b=b: v.tensor_tensor(out=hdb[:, ch, 30:30 + TG], in0=pz[b][:], in1=sg[b][:], op=ALU.mult),
                      r=[("pz", b), ("sg", b)], w=[("hdb", ch)])
                def conv(t, ch=ch):
                    for k in range(31):
                        ins = t.matmul(pc[:], lhsT=diag[:, ch, k, :], rhs=hdb[:, ch, k:k + TG], start=(k == 0), stop=(k == 30))
                    return ins
                S.add("pe", conv, r=[("hdb", ch)] + [("diag", ch, k) for k in range(31)], w=[("pc",)])
                S.add("act", lambda a, ch=ch: a.activation(out=y[:, ch, :], in_=pc[:], func=AF.Identity, bias=oddc[:, ch:ch + 1], scale=1.0),
                      r=[("pc",), ("oddc",)], w=[("y", ch)])
                S.add("pool", lambda p_, ch=ch: p_.tensor_copy(out=hdb[:, ch, 0:30], in_=hdb[:, ch, TG:TG + 30]),
                      r=[("hdb", ch)], w=[("hdb", ch)])
            for i4 in range(4):
                v_pre(i4)
                conv_part(i4)
                v_post(i4)
            for ch in range(4):
                b = ch % 2
                S.add("pe", lambda t, ch=ch: t.matmul(pst[:], lhsT=o512, rhs=y[:, ch, :], start=(ch == 0), stop=(ch == 3)),
                      r=[("y", ch)], w=[("pst",)])
                S.add("act", lambda a, ch=ch, b=b: a.activation(out=tmp[b][:], in_=y[:, ch, :], func=AF.Square), r=[("y", ch)], w=[("tmp", b)])
                S.add("pe", lambda t, ch=ch, b=b: t.matmul(pvt[:], lhsT=o512, rhs=tmp[b][:], start=(ch == 0), stop=(ch == 3)),
                      r=[("tmp", b)], w=[("pvt",)])
            S.add("act", lambda a: a.copy(out=mu[:], in_=pst[:]), r=[("pst",)], w=[("mu",)])
            S.add("dve", lambda v: v.tensor_tensor(out=rs2[:], in0=mu[:], in1=mu[:], op=ALU.mult), r=[("mu",)], w=[("rs2",)])
            S.add("dve", lambda v: v.tensor_tensor(out=rs2[:], in0=pvt[:], in1=rs2[:], op=ALU.subtract), r=[("pvt",), ("rs2",)], w=[("rs2",)])
            S.add("act", lambda a: a.activation(out=rs2[:], in_=rs2[:], func=AF.Sqrt, bias=EPS, scale=1.0), r=[("rs2",)], w=[("rs2",)])
            S.add("dve", lambda v: v.reciprocal(out=rs2[:], in_=rs2[:]), r=[("rs2",)], w=[("rs2",)])
            for ch in range(4):
                b = ch % 2
                S.add("dve", lambda v, ch=ch, b=b: v.tensor_tensor(out=tmp[b][:], in0=y[:, ch, :], in1=mu[:], op=ALU.subtract),
                      r=[("y", ch), ("mu",)], w=[("tmp", b)])
                S.add("dve", lambda v, b=b: v.tensor_tensor(out=tmp[b][:], in0=tmp[b][:], in1=rs2[:], op=ALU.mult),
                      r=[("tmp", b), ("rs2",)], w=[("tmp", b)])
                S.add("act", lambda a, ch=ch, b=b: a.activation(out=cat[:, 4 + ch, :], in_=tmp[b][:], func=AF.Silu,
                                                                bias=oddc[:, 8 + ch:9 + ch], scale=oddc[:, 4 + ch:5 + ch]),
                      r=[("tmp", b), ("oddc",)], w=[("catd", ch)])
            catr = [("cat", s4) for s4 in range(4)] + [("catd", ch) for ch in range(4)]
            for m in range(8):
                b = m % 2
                def mmo(t, m=m, b=b):
                    for c in range(8):
                        ins = t.matmul(pz[b][:], lhsT=Wout[:, c, m * 128:(m + 1) * 128], rhs=cat[:, c, :], start=(c == 0), stop=(c == 7))
                    return ins
                S.add("pe", mmo, r=catr + wor, w=[("pz", b)])
                S.add("dve", lambda v, m=m, b=b, sl=sl: v.tensor_tensor(out=hb[sl][:, m, :], in0=pz[b][:], in1=hb[sl][:, m, :], op=ALU.add),
                      r=[("pz", b), ("h", sl, m)], w=[("h", sl, m)])
            S.dma("pool", hT_d[:, :, g * TG:(g + 1) * TG], hb[sl][:], r=[("h", sl, c) for c in range(8)], w=[("hTd", g)])
        S.emit()

ALL_STAGES = ("tin", "ffn1_0", "evenproj", "dsa", "diff", "wout0", "ffn2_0", "ple0",
              "ffn1_1", "odd", "ffn2_1", "ple1")


def build(S_LEN, stages=ALL_STAGES, dbg=False):
    NG = S_LEN // TG
    nc = bass.Bass("TRN2", target_bir_lowering=False)

    def din(name, shape, dt=F32):
        return nc.dram_tensor(name, shape, dt, kind="ExternalInput").ap()

    def dscr(name, shape, dt):
        return nc.dram_tensor(name, shape, dt, kind="ExternalOutput" if dbg else "Internal").ap()

    x_d = din("x", [S_LEN, D])
    p_d = din("p", [2, S_LEN, 256])
    posb_d = din("posb", [128, S_LEN], I32)
    cst_d = din("consts", [128, NCONST])
    gcols_d = din("gcols", [128, 64])
    hgc_d = din("hgc", [128, 8])
    ffw = {}
    for nm, shp in (("ffn1_wg", [2, D, DFF]), ("ffn1_wu", [2, D, DFF]), ("ffn1_wd", [2, DFF, D]),
                    ("ffn2_wg", [2, D, DFF]), ("ffn2_wu", [2, D, DFF]), ("ffn2_wd", [2, DFF, D])):
        ffw[nm] = din(nm, shp)
    wfm_d = din("w_in0_fm", [D, 2048])
    wtm_d = din("w_in0_tm", [D, 644])
    ev_w_out_d = din("ev_w_out", [1, D, D])
    ple_wgate_d = din("ple_wgate", [2, D, D])
    ple_wproj_d = din("ple_wproj", [2, 256, D])
    a_w_uv_d = din("a_w_uv", [1, 8, 128, 64])
    lamv_d = din("lamv", [128, 4, 64])
    od_w_in_d = din("od_w_in", [1, D, 2048])
    od_w_out_d = din("od_w_out", [1, D, D])
    oddc_d = din("oddc", [128, 16 + 4 * 31])
    oddb_d = din("oddb", [128, 2, 512])
    c_w_s_d = din("c_w_s", [1, 8, 128, 128])
    c_b_s_d = din("c_b_s", [1, 8, 128])
    out_d = nc.dram_tensor("out", [S_LEN, D], F32, kind="ExternalOutput").ap()
    hT_d = dscr("hT", [128, 8, S_LEN], F32)
    qkT_d = dscr("qkT", [16, 128, S_LEN], BF16)
    bv_d = dscr("bv", [S_LEN, 512], BF16)
    av_d = dscr("av", [S_LEN, 128], BF16)
    iw_d = dscr("iw", [S_LEN, 4], F32)
    catT_d = dscr("catT", [8, 128, S_LEN], BF16)
    with ExitStack() as es:
        S = Sched(nc, es)
        cst = T(es, nc, "cst", [128, NCONST], F32)
        gcols = T(es, nc, "gcols_sb", [128, 64], F32)
        hgc = T(es, nc, "hgc_sb", [128, 8], F32)
        S.dma("sp", cst[:], cst_d, w=[("cst",)])
        S.dma("sp", gcols[:], gcols_d, w=[("gcols",)])
        S.dma("sp", hgc[:], hgc_d, w=[("hgc",)])
        S.emit()

        def gc(layer, which):
            o = (layer * 4 + which) * 8
            return gcols[:, o:o + 8]
        if "tin" in stages:
            phase_transpose_in(S, nc, cst, x_d, hT_d, NG)
        if "ffn1_0" in stages:
            phase_ffn(S, nc, cst, gc(0, 0), hT_d, ffw["ffn1_wg"][0], ffw["ffn1_wu"][0], ffw["ffn1_wd"][0], NG)
        if "evenproj" in stages:
            phase_evenproj(S, nc, cst, gc(0, 1), hgc, hT_d, posb_d, wfm_d, wtm_d, qkT_d, bv_d, av_d, iw_d, NG)
        if "dsa" in stages:
            phase_dsa(S, nc, cst, qkT_d, av_d, iw_d, a_w_uv_d[0], catT_d, S_LEN)
        if "diff" in stages:
            phase_diff(S, nc, cst, hgc, lamv_d, qkT_d, bv_d, catT_d, S_LEN)
        if "wout0" in stages:
            phase_proj_res(S, nc, hT_d, catT_d, ev_w_out_d[0], NG)
        if "ffn2_0" in stages:
            phase_ffn(S, nc, cst, gc(0, 2), hT_d, ffw["ffn2_wg"][0], ffw["ffn2_wu"][0], ffw["ffn2_wd"][0], NG)
        if "ple0" in stages:
            phase_ple(S, nc, cst, gc(0, 3), hT_d, p_d[0], ple_wgate_d[0], ple_wproj_d[0], None, NG)
        if "ffn1_1" in stages:
            phase_ffn(S, nc, cst, gc(1, 0), hT_d, ffw["ffn1_wg"][1], ffw["ffn1_wu"][1], ffw["ffn1_wd"][1], NG)
        if "odd" in stages:
            phase_odd(S, nc, cst, gc(1, 1), hT_d, od_w_in_d[0], od_w_out_d[0], oddc_d, oddb_d, c_w_s_d[0], c_b_s_d[0], NG)
        if "ffn2_1" in stages:
            phase_ffn(S, nc, cst, gc(1, 2), hT_d, ffw["ffn2_wg"][1], ffw["ffn2_wu"][1], ffw["ffn2_wd"][1], NG)
        if "ple1" in stages:
            phase_ple(S, nc, cst, gc(1, 3), hT_d, p_d[1], ple_wgate_d[1], ple_wproj_d[1], out_d, NG)
    return nc


def shared_inputs(inputs):
    f = lambda k: np.asarray(inputs[k], np.float32)
    m = {}
    m["consts"] = make_consts()
    g = np.zeros((128, 64), np.float32)
    for layer in range(2):
        for wi, nm in enumerate(("ffn1_g", "mix_g", "ffn2_g", "ple_g")):
            o = (layer * 4 + wi) * 8
            g[:, o:o + 8] = f(nm)[layer].reshape(8, 128).T
    m["gcols"] = g
    hg = np.zeros((128, 8), np.float32)
    for i, nm in enumerate(("a_q_g", "a_k_g", "b_q_g", "b_k_g")):
        hg[:, i] = np.tile(f(nm)[0], 2)
    hg[:, 4] = f("b_subln_g")[0]
    m["hgc"] = hg
    for nm in ("ffn1_wg", "ffn1_wu", "ffn1_wd", "ffn2_wg", "ffn2_wu", "ffn2_wd", "ev_w_out", "ple_wgate",
               "ple_wproj", "a_w_uv", "od_w_in", "od_w_out", "c_w_s", "c_b_s"):
        m[nm] = f(nm)
    w = f("ev_w_in")[0]
    aq, ak, av, iq, ik, iw, bq, bk, bv = (w[:, 0:512], w[:, 512:576], w[:, 576:704], w[:, 704:960], w[:, 960:1024],
                                          w[:, 1024:1028], w[:, 1028:1540], w[:, 1540:2052], w[:, 2052:2564])
    m["w_in0_fm"] = np.ascontiguousarray(np.concatenate([aq, ak, ak, iq, ik, ik, bq, bk], axis=1))
    m["w_in0_tm"] = np.ascontiguousarray(np.concatenate([bv, av, iw], axis=1))
    lam = np.stack([f("b_lam_q1")[0], f("b_lam_k1")[0], f("b_lam_q2")[0], f("b_lam_k2")[0]], axis=0)
    m["lamv"] = np.ascontiguousarray(np.broadcast_to(lam[None], (128, 4, 64)))
    oc = np.zeros((128, 16 + 4 * 31), np.float32)
    for i, nm in enumerate(("d_conv_b", "d_ln_g", "d_ln_b")):
        oc[:, i * 4:(i + 1) * 4] = f(nm)[0].reshape(4, 128).T
    oc[:, 16:] = f("d_conv_w")[0].T.reshape(4, 128, 31).transpose(1, 0, 2).reshape(128, 4 * 31)
    m["oddc"] = oc
    m["oddb"] = np.ascontiguousarray(np.broadcast_to(np.stack([f("c_ln_g")[0], f("c_ln_b")[0]], 0)[None], (128, 2, 512)))
    return m


def host_inputs(inputs, b, S_LEN, shared=None):
    m = dict(shared if shared is not None else shared_inputs(inputs))
    m["x"] = np.ascontiguousarray(np.asarray(inputs["x"])[b, :S_LEN])
    m["p"] = np.ascontiguousarray(np.asarray(inputs["p"])[:, b, :S_LEN])
    m["posb"] = np.ascontiguousarray(np.broadcast_to(np.asarray(inputs["pos"])[b, :S_LEN][None], (128, S_LEN))).astype(np.int32)
    return m


_NC_CACHE = {}


def kernel(**inputs):
    S_LEN = 8192
    if S_LEN not in _NC_CACHE:
        _NC_CACHE[S_LEN] = build(S_LEN)
    nc = _NC_CACHE[S_LEN]
    sh = shared_inputs(inputs)
    in_maps = [host_inputs(inputs, b, S_LEN, sh) for b in range(8)]
    res = run_bass_kernel_spmd(nc, in_maps, core_ids=list(range(8)))
    return np.stack([np.asarray(res.results[b]["out"], np.float32) for b in range(8)], axis=0)
```
